# Optimizing a Trainium2 kernel written in Bass

```python
import jax
import jax.numpy as jnp
from jax import lax
import numpy as np

D_MODEL = 1024
BATCH = 2
SEQ = 8192
DEPTH = 2

GRID_W = 64
CTX_LEN = 256
N_EVEN = (DEPTH + 1) // 2
N_ODD = DEPTH // 2
EPS = 1e-6

NA_HEADS = 8
NA_HEAD_DIM = 64
NA_WIN_R = 8
NA_WIN_C = 16
SG_GROUPS = 8
SG_GROUP_DIM = 64
SG_CHUNK = 128
A_WIDTH = NA_HEADS * NA_HEAD_DIM
B_WIDTH = SG_GROUPS * SG_GROUP_DIM
EVEN_IN = 3 * A_WIDTH + 2 * B_WIDTH
EVEN_MIX = A_WIDTH + B_WIDTH

MLA_HEADS = 16
MLA_NOPE = 64
MLA_ROPE = 32
MLA_V = 64
MLA_Q_RANK = 384
MLA_KV_RANK = 256
MLA_IN = MLA_Q_RANK + MLA_KV_RANK + MLA_ROPE
MLA_SCALE = (MLA_NOPE + MLA_ROPE) ** -0.5
Q_BLOCK = 128
ROPE_BASE = 10000.0

FFN_DIM = 2816
N_EXPERTS = 8
TOP_K = 2
EXPERT_DIM = 3584

kernel_name = 'hybrid_natten_gmlp_mla_moe_dit'


def rms_norm(x, g):
    xf = x.astype(jnp.float32)
    y = xf * lax.rsqrt(jnp.mean(xf * xf, axis=-1, keepdims=True) + EPS)
    return (y * g.astype(jnp.float32)).astype(x.dtype)


def ada_mod(cond, w, b):
    return jnp.split(jax.nn.silu(cond) @ w + b, 6, axis=-1)


def modulate(h, shift, scale):
    return h * (1 + scale[..., None, :]) + shift[..., None, :]


def softmax_f32(s, dtype):
    return jax.nn.softmax(s.astype(jnp.float32), axis=-1).astype(dtype)


def rope_tables(n_tokens, dtype):
    t = jnp.arange(n_tokens, dtype=jnp.int32)
    pos = jnp.stack([t // GRID_W, t % GRID_W], axis=-1).astype(jnp.float32)
    n_freq = MLA_ROPE // 4
    inv = jnp.power(ROPE_BASE, -jnp.arange(n_freq, dtype=jnp.float32) / n_freq)
    ang = pos[:, :, None] * inv
    return jnp.cos(ang).astype(dtype), jnp.sin(ang).astype(dtype)


def apply_rope(x, cos, sin):
    xr = x.reshape(*x.shape[:-1], 2, 2, MLA_ROPE // 4)
    x1, x2 = xr[..., 0, :], xr[..., 1, :]
    out = jnp.stack([x1 * cos - x2 * sin, x1 * sin + x2 * cos], axis=-2)
    return out.reshape(x.shape)


def context_attention(q, k, v):
    s = jnp.einsum('bqhd,bkhd->bhqk', q, k) * (q.shape[-1] ** -0.5)
    o = jnp.einsum('bhqk,bkhd->bqhd', softmax_f32(s, v.dtype), v)
    return o.reshape(*o.shape[:2], -1)


def neighbourhood_attention(q, k, v, k_ctx, v_ctx, rpb):
    B, S, H, Dh = q.shape
    rows = S // GRID_W
    kr = min(NA_WIN_R, rows)
    scale = Dh ** -0.5
    qg = q.reshape(B, rows, GRID_W, H, Dh)
    kg = k.reshape(B, rows, GRID_W, H, Dh)
    vg = v.reshape(B, rows, GRID_W, H, Dh)
    cols = jnp.arange(GRID_W)
    col_start = jnp.clip(cols - NA_WIN_C // 2, 0, GRID_W - NA_WIN_C)
    col_idx = col_start[:, None] + jnp.arange(NA_WIN_C)[None, :]
    col_bias_idx = col_idx - cols[:, None] + (NA_WIN_C - 1)
    n_win = kr * NA_WIN_C

    def one_row(r):
        row_start = jnp.clip(r - kr // 2, 0, rows - kr)
        q_r = lax.dynamic_index_in_dim(qg, r, axis=1, keepdims=False)
        k_rows = lax.dynamic_slice_in_dim(kg, row_start, kr, axis=1)
        v_rows = lax.dynamic_slice_in_dim(vg, row_start, kr, axis=1)
        k_win = k_rows[:, :, col_idx]
        v_win = v_rows[:, :, col_idx]
        row_bias_idx = row_start + jnp.arange(kr) - r + (NA_WIN_R - 1)
        bias = rpb[:, row_bias_idx[None, :, None], col_bias_idx[:, None, :]]
        s_win = jnp.einsum('bwhd,bkwjhd->bhwkj', q_r, k_win) * scale + bias
        s_ctx = jnp.einsum('bwhd,bchd->bhwc', q_r, k_ctx) * scale
        s = jnp.concatenate([s_win.reshape(B, H, GRID_W, n_win), s_ctx], axis=-1)
        p = softmax_f32(s, v.dtype)
        p_win = p[..., :n_win].reshape(B, H, GRID_W, kr, NA_WIN_C)
        o = jnp.einsum('bhwkj,bkwjhd->bwhd', p_win, v_win)
        return o + jnp.einsum('bhwc,bchd->bwhd', p[..., n_win:], v_ctx)

    out = lax.map(one_row, jnp.arange(rows))
    return jnp.moveaxis(out, 0, 1).reshape(B, S, H * Dh)


def spatial_gating(u, g, w_s, b_s, norm_g):
    B, S, _ = u.shape
    u = jax.nn.gelu(u)
    g = jax.nn.gelu(g).reshape(B, S // SG_CHUNK, SG_CHUNK, SG_GROUPS, SG_GROUP_DIM)
    gf = g.astype(jnp.float32)
    mu = jnp.mean(gf, axis=-1, keepdims=True)
    var = jnp.mean(jnp.square(gf - mu), axis=-1, keepdims=True)
    gn = ((gf - mu) * lax.rsqrt(var + EPS)).astype(g.dtype) * norm_g.reshape(SG_GROUPS, SG_GROUP_DIM)
    mixed = jnp.einsum('gpq,bnqgc->bnpgc', w_s, gn) + b_s.T[:, :, None]
    return u * mixed.reshape(B, S, B_WIDTH)


def even_mixer(h_lat, h_ctx, w_in, rpb, w_s, b_s, sg_g, w_out, need_ctx_out):
    heads = lambda t: t.reshape(*t.shape[:-1], NA_HEADS, NA_HEAD_DIM)
    cuts = [A_WIDTH, 2 * A_WIDTH, 3 * A_WIDTH, 3 * A_WIDTH + B_WIDTH]
    q, k, v, u, g = jnp.split(h_lat @ w_in, cuts, axis=-1)
    if need_ctx_out:
        qc, kc, vc, uc, gc = jnp.split(h_ctx @ w_in, cuts, axis=-1)
    else:
        kc, vc = jnp.split(h_ctx @ w_in[:, A_WIDTH:3 * A_WIDTH], 2, axis=-1)
    a_lat = neighbourhood_attention(heads(q), heads(k), heads(v), heads(kc), heads(vc), rpb)
    b_lat = spatial_gating(u, g, w_s, b_s, sg_g)
    y_lat = jnp.concatenate([a_lat, b_lat], axis=-1) @ w_out
    if not need_ctx_out:
        return y_lat, None
    a_ctx = context_attention(heads(qc), heads(kc), heads(vc))
    b_ctx = spatial_gating(uc, gc, w_s, b_s, sg_g)
    y_ctx = jnp.concatenate([a_ctx, b_ctx], axis=-1) @ w_out
    return y_lat, y_ctx


def mla_queries(c_q, q_g, w_uq):
    q = (rms_norm(c_q, q_g) @ w_uq).reshape(*c_q.shape[:-1], MLA_HEADS, MLA_NOPE + MLA_ROPE)
    return q[..., :MLA_NOPE], q[..., MLA_NOPE:]


def mla_keys_values(c_kv, kv_g, w_ukv):
    kv = (rms_norm(c_kv, kv_g) @ w_ukv).reshape(*c_kv.shape[:-1], MLA_HEADS, MLA_NOPE + MLA_V)
    return kv[..., :MLA_NOPE], kv[..., MLA_NOPE:]


def mla_scores_out(qn, qr, kn, kr, v):
    s = (jnp.einsum('bqhd,bkhd->bhqk', qn, kn) + jnp.einsum('bqhr,bkr->bhqk', qr, kr)) * MLA_SCALE
    return jnp.einsum('bhqk,bkhd->bqhd', softmax_f32(s, v.dtype), v)


def mla_latent_attention(q_nope, q_rope, k_nope, k_rope, v):
    B, S = q_nope.shape[:2]
    nb = S // Q_BLOCK
    blocks = lambda t: jnp.moveaxis(t.reshape(B, nb, Q_BLOCK, *t.shape[2:]), 1, 0)
    out = lax.map(lambda qs: mla_scores_out(qs[0], qs[1], k_nope, k_rope, v), (blocks(q_nope), blocks(q_rope)))
    return jnp.moveaxis(out, 0, 1).reshape(B, S, MLA_HEADS * MLA_V)


def odd_mixer(h_lat, h_ctx, w_in, q_g, kv_g, w_uq, w_ukv, w_out, cos, sin, need_ctx_out):
    cuts = [MLA_Q_RANK, MLA_Q_RANK + MLA_KV_RANK]
    c_q, c_kv, k_rope = jnp.split(h_lat @ w_in, cuts, axis=-1)
    q_nope, q_rope = mla_queries(c_q, q_g, w_uq)
    q_rope = apply_rope(q_rope, cos[:, None], sin[:, None])
    k_rope = apply_rope(k_rope, cos, sin)
    k_nope, v = mla_keys_values(c_kv, kv_g, w_ukv)
    if need_ctx_out:
        cq_c, ckv_c, kr_c = jnp.split(h_ctx @ w_in, cuts, axis=-1)
    else:
        ckv_c, kr_c = jnp.split(h_ctx @ w_in[:, MLA_Q_RANK:], [MLA_KV_RANK], axis=-1)
    kn_c, v_c = mla_keys_values(ckv_c, kv_g, w_ukv)
    k_nope_all = jnp.concatenate([k_nope, kn_c], axis=1)
    k_rope_all = jnp.concatenate([k_rope, kr_c], axis=1)
    v_all = jnp.concatenate([v, v_c], axis=1)
    y_lat = mla_latent_attention(q_nope, q_rope, k_nope_all, k_rope_all, v_all) @ w_out
    if not need_ctx_out:
        return y_lat, None
    qn_c, qr_c = mla_queries(cq_c, q_g, w_uq)
    o_c = mla_scores_out(qn_c, qr_c, kn_c, kr_c, v_c)
    y_ctx = o_c.reshape(*o_c.shape[:2], MLA_HEADS * MLA_V) @ w_out
    return y_lat, y_ctx


def swiglu(h, w_gate, w_up, w_down):
    return (jax.nn.silu(h @ w_gate) * (h @ w_up)) @ w_down


def moe_swiglu(h, w_router, b_router, w_gate, w_up, w_down):
    logits = (h @ w_router + b_router).astype(jnp.float32)
    top_v, top_i = lax.top_k(logits, TOP_K)
    weights = jax.nn.softmax(top_v, axis=-1)
    combine = jnp.sum(jax.nn.one_hot(top_i, N_EXPERTS, dtype=jnp.float32) * weights[..., None], axis=-2).astype(h.dtype)
    y = jnp.zeros_like(h)
    for e in range(N_EXPERTS):
        y = y + combine[..., e:e + 1] * swiglu(h, w_gate[e], w_up[e], w_down[e])
    return y


def setup_inputs(seed: int = 0) -> dict:
    key = jax.random.key(seed)
    ks = iter(jax.random.split(key, 32))
    nrm = lambda shape, scale: jax.random.normal(next(ks), shape, jnp.float32) * scale
    gain = lambda shape: 1.0 + nrm(shape, 0.05)
    D = D_MODEL
    return {
        'x': nrm((BATCH, SEQ, D), 1.0),
        'c': nrm((BATCH, D), 1.0),
        'ctx': nrm((BATCH, CTX_LEN, D), 1.0),
        'c_ctx': nrm((D,), 1.0),
        'ada_w': nrm((DEPTH, D, 6 * D), 0.5 * D ** -0.5),
        'ada_b': nrm((DEPTH, 6 * D), 0.02),
        'norm_g': gain((DEPTH, 2, D)),
        'final_g': gain((D,)),
        'na_w_in': nrm((N_EVEN, D, EVEN_IN), D ** -0.5),
        'na_rpb': nrm((N_EVEN, NA_HEADS, 2 * NA_WIN_R - 1, 2 * NA_WIN_C - 1), 0.5),
        'sg_w': nrm((N_EVEN, SG_GROUPS, SG_CHUNK, SG_CHUNK), SG_CHUNK ** -0.5),
        'sg_b': nrm((N_EVEN, SG_GROUPS, SG_CHUNK), 0.02),
        'sg_norm_g': gain((N_EVEN, B_WIDTH)),
        'even_w_out': nrm((N_EVEN, EVEN_MIX, D), EVEN_MIX ** -0.5),
        'ffn_w_gate': nrm((N_EVEN, D, FFN_DIM), D ** -0.5),
        'ffn_w_up': nrm((N_EVEN, D, FFN_DIM), D ** -0.5),
        'ffn_w_down': nrm((N_EVEN, FFN_DIM, D), FFN_DIM ** -0.5),
        'mla_w_in': nrm((N_ODD, D, MLA_IN), D ** -0.5),
        'mla_q_norm_g': gain((N_ODD, MLA_Q_RANK)),
        'mla_kv_norm_g': gain((N_ODD, MLA_KV_RANK)),
        'mla_w_uq': nrm((N_ODD, MLA_Q_RANK, MLA_HEADS * (MLA_NOPE + MLA_ROPE)), MLA_Q_RANK ** -0.5),
        'mla_w_ukv': nrm((N_ODD, MLA_KV_RANK, MLA_HEADS * (MLA_NOPE + MLA_V)), MLA_KV_RANK ** -0.5),
        'mla_w_out': nrm((N_ODD, MLA_HEADS * MLA_V, D), (MLA_HEADS * MLA_V) ** -0.5),
        'moe_w_router': nrm((N_ODD, D, N_EXPERTS), D ** -0.5),
        'moe_b_router': nrm((N_ODD, N_EXPERTS), 0.01),
        'moe_w_gate': nrm((N_ODD, N_EXPERTS, D, EXPERT_DIM), D ** -0.5),
        'moe_w_up': nrm((N_ODD, N_EXPERTS, D, EXPERT_DIM), D ** -0.5),
        'moe_w_down': nrm((N_ODD, N_EXPERTS, EXPERT_DIM, D), EXPERT_DIM ** -0.5),
    }


def reference(x, c, ctx, c_ctx, ada_w, ada_b, norm_g, final_g, na_w_in, na_rpb, sg_w, sg_b, sg_norm_g,
              even_w_out, ffn_w_gate, ffn_w_up, ffn_w_down, mla_w_in, mla_q_norm_g, mla_kv_norm_g,
              mla_w_uq, mla_w_ukv, mla_w_out, moe_w_router, moe_b_router, moe_w_gate, moe_w_up, moe_w_down):
    S = x.shape[1]
    n_ctx = ctx.shape[1]
    cos, sin = rope_tables(S, x.dtype)
    for layer in range(DEPTH):
        last = layer == DEPTH - 1
        i = layer // 2
        sh_m, sc_m, g_m, sh_f, sc_f, g_f = ada_mod(c, ada_w[layer], ada_b[layer])
        csh_m, csc_m, cg_m, csh_f, csc_f, cg_f = ada_mod(c_ctx, ada_w[layer], ada_b[layer])
        h_lat = modulate(rms_norm(x, norm_g[layer, 0]), sh_m, sc_m)
        h_ctx = modulate(rms_norm(ctx, norm_g[layer, 0]), csh_m, csc_m)
        if layer % 2 == 0:
            y_lat, y_ctx = even_mixer(h_lat, h_ctx, na_w_in[i], na_rpb[i], sg_w[i], sg_b[i], sg_norm_g[i],
                                      even_w_out[i], not last)
            ffn = lambda h: swiglu(h, ffn_w_gate[i], ffn_w_up[i], ffn_w_down[i])
        else:
            y_lat, y_ctx = odd_mixer(h_lat, h_ctx, mla_w_in[i], mla_q_norm_g[i], mla_kv_norm_g[i], mla_w_uq[i],
                                     mla_w_ukv[i], mla_w_out[i], cos, sin, not last)
            ffn = lambda h: moe_swiglu(h, moe_w_router[i], moe_b_router[i], moe_w_gate[i], moe_w_up[i], moe_w_down[i])
        x = x + g_m[:, None, :] * y_lat
        h_lat = modulate(rms_norm(x, norm_g[layer, 1]), sh_f, sc_f)
        if last:
            x = x + g_f[:, None, :] * ffn(h_lat)
        else:
            ctx = ctx + cg_m[None, :] * y_ctx
            h_ctx = modulate(rms_norm(ctx, norm_g[layer, 1]), csh_f, csc_f)
            f = ffn(jnp.concatenate([h_ctx, h_lat], axis=1))
            ctx = ctx + cg_f[None, :] * f[:, :n_ctx]
            x = x + g_f[:, None, :] * f[:, n_ctx:]
    return rms_norm(x, final_g)
```

```python
import numpy as np
import concourse.bass as bass
import concourse.mybir as mybir
from concourse.bass_utils import run_bass_kernel_spmd

F32 = mybir.dt.float32
BF16 = mybir.dt.bfloat16
AF = mybir.ActivationFunctionType
ALU = mybir.AluOpType
AX = mybir.AxisListType
DTSZ = {F32: 4, BF16: 2}
NSLOT = 24

GRID_W = 64
_NOCC = 0
_CCFIRST = None
EPS = 1e-6


class Op:
    __slots__ = ("eng", "fn", "dom", "idx", "waits", "signal", "clock", "is_dma", "cnt", "step")


class Rec:
    def __init__(self, nc):
        self.nc = nc
        self.ops = []
        self.meta = {}
        self.live = {}
        self.known = {}
        self.dom_ops = {}
        self.slot_rr = {}
        self.slot_last = {}

    def sb(self, name, shape, dt):
        t = self.nc.alloc_sbuf_tensor(name, list(shape), dt)
        self.meta[name] = ("sb", int(np.prod(shape[1:])) * DTSZ[dt])
        return t

    def ps(self, name, shape, dt=F32):
        t = self.nc.alloc_psum_tensor(name, list(shape), dt)
        self.meta[name] = ("ps", int(np.prod(shape[1:])) * DTSZ[dt])
        return t

    def dram(self, name, shape, dt, kind="Internal"):
        t = self.nc.dram_tensor(name, list(shape), dt, kind=kind)
        self.meta[name] = ("dram", 0)
        return t

    def box(self, ap):
        name = ap.tensor.name
        kind, row = self.meta[name]
        sz = DTSZ[ap.dtype]
        off = int(ap.offset) * sz
        dims = ap.ap
        if kind == "dram":
            hi = off + sum((c - 1) * abs(s) for s, c in dims) * sz + sz - 1
            return (name, 0, 0, off, hi)
        p0 = off // row
        f0 = off % row
        ps_, pc = dims[0]
        pstep = (abs(ps_) * sz) // row if pc > 1 else 0
        p1 = p0 + (pc - 1) * pstep
        f1 = f0 + sum((c - 1) * abs(s) for s, c in dims[1:]) * sz + sz - 1
        return (name, p0, p1, f0, f1)

    @staticmethod
    def _ovl(a, b):
        return a[1] <= b[2] and b[1] <= a[2] and a[3] <= b[4] and b[3] <= a[4]

    @staticmethod
    def _cov(a, b):
        return a[1] <= b[1] and a[2] >= b[2] and a[3] <= b[3] and a[4] >= b[4]

    def op(self, eng, fn, reads=(), writes=(), dma=False, dom=None, step=None):
        o = Op()
        o.eng = eng
        o.fn = fn
        o.is_dma = dma
        o.signal = False
        o.waits = {}
        E = eng
        known = self.known.setdefault(E, {})
        deps = {}
        rb = [self.box(a) for a in reads]
        wb = [self.box(a) for a in writes]
        psb = set()
        for lst_ in (rb, wb):
            for i_, b in enumerate(lst_):
                if self.meta[b[0]][0] == "ps":
                    lst_[i_] = (b[0], 0, 127, (b[3] // 2048) * 2048, (b[4] // 2048) * 2048 + 2047)
                    psb.add(b[0])
        for b in rb:
            ps_ = b[0] in psb
            for (lb, lop, lw) in self.live.get(b[0], ()):
                if self._ovl(b, lb):
                    if lw:
                        deps[lop] = True
                    elif ps_ and lop.eng != E:
                        deps[lop] = True
        for b in wb:
            for (lb, lop, lw) in self.live.get(b[0], ()):
                if self._ovl(b, lb):
                    deps[lop] = deps.get(lop, False) or lw
        if dma:
            rr = self.slot_rr.get(E, 0)
            self.slot_rr[E] = rr + 1
            dom = ("dma", E, rr % NSLOT)
            prev = self.slot_last.get(dom)
            if prev is not None:
                deps[prev] = True
            self.slot_last[dom] = o
        elif dom is None:
            dom = E
        o.dom = dom
        o.step = step if step is not None else (16 if dma else 1)
        for y, hard in deps.items():
            if y.dom == E:
                if E == "pe" or not hard:
                    continue
            if known.get(y.dom, -1) >= y.idx:
                continue
            if o.waits.get(y.dom, (-1, None))[0] < y.idx:
                o.waits[y.dom] = (y.idx, y)
        for d, (i, y) in o.waits.items():
            y.signal = True
            for k, v in y.clock.items():
                if known.get(k, -1) < v:
                    known[k] = v
        lst = self.dom_ops.setdefault(dom, [])
        o.idx = len(lst)
        lst.append(o)
        o.clock = dict(known)
        o.clock[dom] = o.idx
        for b in wb:
            l = self.live.setdefault(b[0], [])
            l[:] = [x for x in l if not self._cov(b, x[0])]
            l.append((b, o, True))
        for b in rb:
            l = self.live.setdefault(b[0], [])
            l[:] = [x for x in l if not (x[1].dom == dom and not x[2] and x[0] == b)]
            l.append((b, o, False))
        self.ops.append(o)
        return o

    def mm(self, out, lhsT, rhs, start=True, stop=True):
        return self.op("pe", lambda e: e.matmul(out, lhsT, rhs, start=start, stop=stop), [lhsT, rhs], [out])

    def tr(self, out, in_, ident):
        return self.op("pe", lambda e: e.transpose(out, in_, ident), [in_, ident], [out])

    def act(self, out, in_, func, bias=None, scale=None, accum_out=None):
        kw = {}
        rd = [in_]
        wr = [out]
        if bias is not None:
            kw["bias"] = bias
            if not isinstance(bias, (int, float)):
                rd.append(bias)
        if scale is not None:
            kw["scale"] = scale
            if not isinstance(scale, (int, float)):
                rd.append(scale)
        if accum_out is not None:
            kw["accum_out"] = accum_out
            wr.append(accum_out)
        return self.op("act", lambda e: e.activation(out, in_, func, **kw), rd, wr)

    def tt(self, eng, out, in0, in1, op):
        return self.op(eng, lambda e: e.tensor_tensor(out, in0, in1, op), [in0, in1], [out])

    def ts(self, eng, out, in0, s1, s2, op0, op1=None, accum_out=None):
        rd = [in0] + [s for s in (s1, s2) if s is not None and not isinstance(s, (int, float))]
        wr = [out] + ([accum_out] if accum_out is not None else [])
        kw = {}
        if op1 is not None:
            kw["op1"] = op1
        if accum_out is not None:
            kw["accum_out"] = accum_out
        return self.op(eng, lambda e: e.tensor_scalar(out, in0, s1, s2, op0, **kw), rd, wr)

    def stt(self, eng, out, in0, scalar, in1, op0, op1):
        rd = [in0, in1] + ([scalar] if not isinstance(scalar, (int, float)) else [])
        return self.op(eng, lambda e: e.scalar_tensor_tensor(out, in0, scalar, in1, op0, op1), rd, [out])

    def cp(self, eng, out, in_):
        if eng == "act":
            return self.op(eng, lambda e: e.copy(out, in_), [in_], [out])
        return self.op(eng, lambda e: e.tensor_copy(out, in_), [in_], [out])

    def memset(self, eng, out, val):
        return self.op(eng, lambda e: e.memset(out, val), [], [out])

    def recip(self, out, in_):
        return self.op("dve", lambda e: e.reciprocal(out, in_), [in_], [out])

    def red(self, out, in_, op):
        return self.op("dve", lambda e: e.tensor_reduce(out, in_, AX.X, op), [in_], [out])

    def dma(self, q, out, in_, **kw):
        return self.op(q, lambda e: e.dma_start(out, in_, **kw), [in_], [out], dma=True)

    def emit(self):
        nc = self.nc
        tails = {}
        for dom, last in self.slot_last.items():
            tails.setdefault(dom[1], []).append(last)
            last.signal = True
        sems = {}
        for dom, lst in self.dom_ops.items():
            nm = "s_" + ("_".join(str(x) for x in dom) if isinstance(dom, tuple) else dom)
            sems[dom] = nc.alloc_semaphore(nm)
            c = 0
            for o in lst:
                if o.signal:
                    c += o.step
                o.cnt = c
        per_eng = {}
        for o in self.ops:
            per_eng.setdefault(o.eng, []).append(o)
        engmap = {"pe": "tensor", "act": "scalar", "dve": "vector", "pool": "gpsimd", "sp": "sync"}
        with nc.Block() as block:
            for E, lst in per_eng.items():
                def body(e, lst=lst, E=E):
                    for o in lst:
                        for d, (i, y) in o.waits.items():
                            e.wait_ge(sems[d], y.cnt)
                        ins = o.fn(e)
                        if o.signal:
                            ins.then_inc(sems[o.dom], o.step)
                    for last in tails.get(E, ()):
                        e.wait_ge(sems[last.dom], last.cnt)
                getattr(block, engmap[E])(body)


class Arena:
    def __init__(self, R, cap):
        self.t = R.sb("arena", [128, cap // 2], BF16)
        self.free = [(0, cap)]
        self.used = {}

    def alloc(self, shape, dt, tag=None):
        n = int(np.prod(shape)) * DTSZ[dt]
        na = (n + 63) // 64 * 64
        for i, (o, s) in enumerate(self.free):
            if s >= na:
                self.free[i] = (o + na, s - na)
                break
        else:
            raise MemoryError(f"arena full: want {na} free {self.free} tag {tag}")
        ap = self.t[:, o // 2:(o + n) // 2]
        if dt != BF16:
            ap = ap.bitcast(dt)
        if len(shape) == 2:
            ap = ap.rearrange("p (a b) -> p a b", b=shape[1])
        elif len(shape) == 3:
            ap = ap.rearrange("p (a b c) -> p a b c", b=shape[1], c=shape[2])
        self.used[id(ap)] = (o, na)
        ap_key = ap
        self._last = (o, na)
        return ap

    def rel(self, ap):
        o, na = self.used.pop(id(ap))
        self.free.append((o, na))
        self.free.sort()
        m = []
        for o, s in self.free:
            if m and m[-1][0] + m[-1][1] == o:
                m[-1] = (m[-1][0], m[-1][1] + s)
            else:
                m.append((o, s))
        self.free = [x for x in m if x[1] > 0]


def build(which, stop_after=None, taps=()):
    nc = bass.Bass("TRN2", target_bir_lowering=False)
    R = Rec(nc)
    A = Arena(R, 206 * 1024)
    PSP = [R.ps(f"psp{i}", [128, 1024], F32) for i in range(4)]
    PS = [PSP[i // 2][:, (i % 2) * 512:(i % 2 + 1) * 512] for i in range(8)]
    PSB = [p.bitcast(BF16) for p in PS]
    tap_out = {}

    def din(name, shape):
        return R.dram(name, shape, F32, kind="ExternalInput")

    L0 = which in ("l0", "fused")
    L1 = which in ("l1", "fused")
    FUSED = which == "fused"
    d0 = lambda n, sh: din(n, sh) if L0 else None
    d1 = lambda n, sh: din(n, sh) if L1 else None
    xh = d0("xh", [2560, 1024])
    ctx = din("ctx", [256, 1024])
    cond = din("cond", [2, 1024])
    ada_w = din("ada_w", [2, 1024, 6144])
    ada_b = din("ada_b", [2, 6144])
    norm_g = din("norm_g", [2, 2, 1024])
    final_g = d1("final_g", [1024])
    na_w_in = d0("na_w_in", [1024, 2560])
    nabias = d0("nabias", [8, 128, 27, 128])
    sg_w = d0("sg_w", [8, 128, 128])
    sg_b = d0("sg_b", [8, 128])
    sg_norm_g = d0("sg_norm_g", [512])
    even_w_out = d0("even_w_out", [1024, 1024])
    ffn_w_gate = d0("ffn_w_gate", [1024, 2816])
    ffn_w_up = d0("ffn_w_up", [1024, 2816])
    ffn_w_down = d0("ffn_w_down", [2816, 1024])
    xb = din("xb", [8192, 1024]) if which == "l1" else None
    xo = din("xo", [2048, 1024]) if which == "l1" else None
    mla_w_in = d1("mla_w_in", [1024, 672])
    mla_w_kr_sw = d1("mla_w_kr_sw", [1024, 32])
    mla_q_norm_g = d1("mla_q_norm_g", [384])
    mla_kv_norm_g = d1("mla_kv_norm_g", [256])
    mla_w_uq = d1("mla_w_uq", [384, 1536])
    mla_w_uq_sw = d1("mla_w_uq_sw", [384, 1536])
    mla_w_ukv = d1("mla_w_ukv", [256, 2048])
    mla_w_out = d1("mla_w_out", [1024, 1024])
    moe_w_router = d1("moe_w_router", [1024, 8])
    moe_b_router = d1("moe_b_router", [8])
    moe_w_gate = d1("moe_w_gate", [8, 1024, 3584])
    moe_w_up = d1("moe_w_up", [8, 1024, 3584])
    moe_w_down = d1("moe_w_down", [8, 3584, 1024])
    ropeq = d1("ropeq", [2, 96, 2048])
    ropek = d1("ropek", [2, 32, 2048 if FUSED else 8192])
    ident_d = din("ident", [128, 128])
    if which == "l0":
        x2_d = R.dram("x2", [2048, 1024], F32, kind="ExternalOutput")
        c2_d = R.dram("c2", [256, 1024], F32, kind="ExternalOutput")
    else:
        out_d = R.dram("out", [2048, 1024], F32, kind="ExternalOutput")

    def tap(name, ap_sb, shape):
        if name not in taps:
            return
        t = R.dram("tap_" + name, [128] + list(shape), ap_sb.dtype, kind="ExternalOutput")
        tap_out[name] = t
        R.dma("sp", t.ap(), ap_sb)

    ident32 = A.alloc([128], F32)
    ident16 = A.alloc([128], BF16)
    ones32 = A.alloc([128], F32)
    modv = [A.alloc([48, 2], F32) for _ in range(2)]
    AB = [[A.alloc([8, 2], F32) for _ in range(4)] for _ in range(2)]
    R.dma("sp", ident32, ident_d.ap())
    R.cp("dve", ident16, ident32)
    ones16 = A.alloc([128], BF16)
    R.memset("dve", ones32, 1.0)
    R.memset("dve", ones16, 1.0)

    def dbg_cc(pos):
        if _CCFIRST != pos:
            return
        d_s = R.dram("dbg_s", [32, 2048], BF16)
        d_d = R.dram("dbg_d", [128, 2048], BF16)
        R.dma("sp", d_s.ap()[:, 0:128], ident16[0:32, :])
        R.op("pool", lambda e: e.collective_compute(
            "AllGather", ALU.bypass, replica_groups=[[0, 1, 2, 3], [4, 5, 6, 7]], ins=[d_s.ap()], outs=[d_d.ap()]),
            [d_s.ap()], [d_d.ap()], dom="ccx", step=1)
        R.dma("sp", ones32[0:32, 0:64].bitcast(BF16), d_d.ap()[32:64, 0:128])
        R.memset("dve", ones32, 1.0)

    dbg_cc("A")
    condT = A.alloc([8, 2], F32)
    for r_ in range(2):
        R.dma("sp", condT[:, :, r_], cond.ap()[r_].rearrange("(k p) -> p k", p=128), allow_slow_non_contiguous=True)
    R.act(condT, condT, AF.Silu)
    def ada_cb(l, cb, wt, psA):
        R.dma("sp", wt, ada_w.ap()[l, :, cb * 512:(cb + 1) * 512].rearrange("(k p) c -> p k c", p=128))
        for fc in range(4):
            j = cb * 4 + fc
            for k in range(8):
                R.mm(psA[:, j * 2:j * 2 + 2], wt[:, k, fc * 128:(fc + 1) * 128],
                     condT[:, k, :], start=(k == 0), stop=(k == 7))

    def ada_finish(l, psA):
        adab = A.alloc([48], F32)
        ng = A.alloc([2, 8], F32)
        R.dma("sp", adab, ada_b.ap()[l].rearrange("(j p) -> p j", p=128), allow_slow_non_contiguous=True)
        for w_ in range(2):
            R.dma("sp", ng[:, w_, :], norm_g.ap()[l, w_].rearrange("(k p) -> p k", p=128), allow_slow_non_contiguous=True)
        R.tt("dve", modv[l], psA[:, 0:96].rearrange("p (j c) -> p j c", c=2),
             adab.rearrange("p (j o) -> p j o", o=1).broadcast_to([128, 48, 2]), ALU.add)
        for w in range(2):
            sh = modv[l][:, w * 24:w * 24 + 8, :]
            sc = modv[l][:, w * 24 + 8:w * 24 + 16, :]
            Am, Bm = AB[l][2 * w], AB[l][2 * w + 1]
            R.ts("dve", Am, sc, 1.0, None, ALU.add)
            R.tt("dve", Am, Am, ng[:, w, :].rearrange("p (k o) -> p k o", o=1).broadcast_to([128, 8, 2]), ALU.mult)
            R.cp("dve", Bm, sh)
        A.rel(adab); A.rel(ng)

    stg = [A.alloc([8, 512], F32) for _ in range(2)]
    for l in ((0,) if L0 else (1,)):
        for cb in range(12):
            ada_cb(l, cb, stg[cb % 2], PS[0])
        ada_finish(l, PS[0])
    A.rel(stg[0]); A.rel(stg[1])
    if not FUSED:
        A.rel(condT)
    tap("modv0", modv[0], [48, 2])
    tap("AB0", AB[0][0], [8, 2])

    def gate_bc(dst, l, which, c):
        dg = A.alloc([2, 128], F32)
        for k in range(8):
            d = dg[:, k % 2, :]
            R.ts("dve", d, ident32, modv[l][:, which * 8 + k, c:c + 1], None, ALU.mult)
            pb = PS[6 + (k // 4)]
            R.mm(pb[:, (k % 4) * 128:(k % 4 + 1) * 128], ones32, d)
        R.cp("act", dst[:, 0:512], PS[6][:, :])
        R.cp("act", dst[:, 512:1024], PS[7][:, :])
        A.rel(dg)

    def norm_group(xs, Aap, Bap, c, HT, col0, want32=None):
        n = len(xs)
        ss = A.alloc([4], F32)
        rs = A.alloc([4], F32)
        junk = A.alloc([1024], BF16)
        xn = [A.alloc([1024], BF16) for _ in range(n)]
        for i, x in enumerate(xs):
            R.act(junk, x, AF.Square, accum_out=ss[:, i:i + 1])
        R.act(rs[:, 0:n], ss[:, 0:n], AF.Sqrt, scale=1.0 / 1024, bias=EPS)
        R.recip(rs[:, 0:n], rs[:, 0:n])
        for i, x in enumerate(xs):
            R.ts("dve" if i % 2 == 0 else "pool", xn[i], x, rs[:, i:i + 1], None, ALU.mult)
        for k in range(8):
            pst = PSB[4 + (k % 2)][:, 0:n * 128]
            for i in range(n):
                R.tr(pst[:, i * 128:(i + 1) * 128], xn[i][:, k * 128:(k + 1) * 128], ident16)
            dst = HT[:, k, col0:col0 + n * 128]
            if k % 2 == 0:
                R.ts("dve", dst, pst, Aap[:, k, c:c + 1], Bap[:, k, c:c + 1], ALU.mult, ALU.add)
            else:
                R.act(dst, pst, AF.Identity, scale=Aap[:, k, c:c + 1], bias=Bap[:, k, c:c + 1])
        for t in xn:
            A.rel(t)
        A.rel(junk); A.rel(rs); A.rel(ss)

    _cm = {}

    def alloc_common():
        XC = A.alloc([2, 1024], F32, "XC")
        XO = A.alloc([16, 1024], F32, "XO")

        def Xt(t):
            return XC[:, t, :] if t < 2 else XO[:, t - 2, :]

        gb = [A.alloc([1024], F32) for _ in range(2)]
        tmp = [A.alloc([1024], F32) for _ in range(2)]
        _cm.update(XC=XC, XO=XO, Xt=Xt, gb=gb, tmp=tmp)
        return XC, XO, Xt, gb, tmp

    def ffn(HT, ncols, tiles, wg_ap, wu_ap, wd_ap, nf, gbcs, comb=None):
        GS = 4
        ngrp = (nf + GS - 1) // GS
        Wg = [A.alloc([8, GS * 128], BF16) for _ in range(2)]
        Wu = [A.alloc([8, GS * 128], BF16) for _ in range(2)]
        Wd = [A.alloc([GS, 1024], BF16) for _ in range(2)]
        AT = A.alloc([GS, ncols], BF16, "AT")
        Sg = [A.alloc([512], BF16) for _ in range(2)]
        blocks = [(b, min(512, ncols - b)) for b in range(0, ncols, 512)]
        ev = 0
        for gi in range(ngrp):
            f0 = gi * GS
            gs = min(GS, nf - f0)
            wg, wu, wd = Wg[gi % 2], Wu[gi % 2], Wd[gi % 2]
            R.dma("pool", wg[:, :, 0:gs * 128], wg_ap[:, f0 * 128:(f0 + gs) * 128].rearrange("(k p) c -> p k c", p=128))
            R.dma("pool", wu[:, :, 0:gs * 128], wu_ap[:, f0 * 128:(f0 + gs) * 128].rearrange("(k p) c -> p k c", p=128))
            R.dma("pool", wd[:, 0:gs, :], wd_ap[f0 * 128:(f0 + gs) * 128, :].rearrange("(k p) c -> p k c", p=128))
            for (c0, cn) in blocks:
                for fc in range(gs):
                    pg, pu = PS[(ev % 2) * 2], PS[(ev % 2) * 2 + 1]
                    sgt = Sg[ev % 2]
                    ev += 1
                    for k in range(8):
                        R.mm(pg[:, 0:cn], wg[:, k, fc * 128:(fc + 1) * 128], HT[:, k, c0:c0 + cn], start=(k == 0), stop=(k == 7))
                    for k in range(8):
                        R.mm(pu[:, 0:cn], wu[:, k, fc * 128:(fc + 1) * 128], HT[:, k, c0:c0 + cn], start=(k == 0), stop=(k == 7))
                    R.act(sgt[:, 0:cn], pg[:, 0:cn], AF.Silu)
                    R.tt("dve", AT[:, fc, c0:c0 + cn], sgt[:, 0:cn], pu[:, 0:cn], ALU.mult)
            for (t, col0, gidx) in tiles:
                for nh in range(2):
                    pb = PS[4 + nh + 2 * (t % 2)]
                    for fc in range(gs):
                        R.mm(pb[:, :], AT[:, fc, col0:col0 + 128], wd[:, fc, nh * 512:(nh + 1) * 512], start=(fc == 0), stop=(fc == gs - 1))
                    tm = _cm['tmp'][t % 2][:, nh * 512:(nh + 1) * 512]
                    xs = _cm['Xt'](t)[:, nh * 512:(nh + 1) * 512]
                    R.tt("dve", tm, pb[:, :], gbcs[gidx][:, nh * 512:(nh + 1) * 512], ALU.mult)
                    if comb is None:
                        R.tt("pool", xs, xs, tm, ALU.add)
                    else:
                        R.stt("dve", xs, tm, comb(t), xs, ALU.mult, ALU.add)
        for x in Wg + Wu + Wd + Sg + [AT]:
            A.rel(x)

    if L0:
        HT0 = A.alloc([8, 2816], BF16, "HT0")
        xt = [A.alloc([1024], F32) for _ in range(4)]
        for t in range(2):
            R.dma("sp", xt[t], ctx.ap()[t * 128:(t + 1) * 128, :])
        norm_group(xt[0:2], AB[0][0], AB[0][1], 1, HT0, 0)
        for g in range(5):
            for i in range(4):
                t = g * 4 + i
                R.dma("sp", xt[i], xh.ap()[t * 128:(t + 1) * 128, :])
            norm_group(xt, AB[0][0], AB[0][1], 0, HT0, 256 + g * 512)
        for t in xt:
            A.rel(t)
        tap("HT0", HT0, [8, 2816])
        dbg_cc("B")
        if stop_after == "B":
            R.emit()
            return nc, tap_out

        QT = A.alloc([4, 2816], BF16, "QT")
        KT = A.alloc([4, 2816], BF16, "KT")
        VA = A.alloc([22, 8 * 65], BF16, "VA")
        MIX = A.alloc([18, 1024], BF16, "MIX")
        R.memset("pool", VA, 1.0)
        blocks = [(b * 512, 512) for b in range(5)] + [(2560, 256)]
        W = A.alloc([8, 1024], BF16, "Wqk")
        R.dma("pool", W, na_w_in.ap()[:, 0:1024].rearrange("(k p) c -> p k c", p=128))
        ev = 0
        for which, DST in ((0, QT), (1, KT)):
            for j in range(4):
                for (c0, cn) in blocks:
                    pb = PS[ev % 4]
                    for k in range(8):
                        R.mm(pb[:, 0:cn], W[:, k, which * 512 + j * 128: which * 512 + (j + 1) * 128],
                             HT0[:, k, c0:c0 + cn], start=(k == 0), stop=(k == 7))
                    if which == 0:
                        R.act(DST[:, j, c0:c0 + cn], pb[:, 0:cn], AF.Identity, scale=0.125)
                    else:
                        R.cp("dve", DST[:, j, c0:c0 + cn], pb[:, 0:cn])
                    ev += 1
        A.rel(W)
        W = A.alloc([8, 512], BF16, "Wv")
        R.dma("pool", W, na_w_in.ap()[:, 1024:1536].rearrange("(k p) c -> p k c", p=128))
        for t in range(22):
            pb = PS[ev % 4]
            ev += 1
            for k in range(8):
                R.mm(pb[:, :], HT0[:, k, t * 128:(t + 1) * 128], W[:, k, :], start=(k == 0), stop=(k == 7))
            dst = VA[:, t, :].rearrange("p (h d) -> p h d", d=65)[:, :, 0:64]
            R.cp("dve" if t % 2 == 0 else "act", dst, pb[:, :].rearrange("p (h d) -> p h d", d=64))
        A.rel(W)
        tap("QT", QT, [4, 2816]); tap("KT", KT, [4, 2816]); tap("VA", VA, [22, 520])

        W = A.alloc([8, 1024], BF16, "Wug")
        R.dma("pool", W, na_w_in.ap()[:, 1536:2560].rearrange("(k p) c -> p k c", p=128))
        ws32 = A.alloc([8, 128], F32)
        ws16 = A.alloc([8, 128], BF16)
        WST = A.alloc([8, 128], BF16)
        R.dma("sp", ws32, sg_w.ap().rearrange("g p q -> p g q"))
        R.cp("dve", ws16, ws32)
        for g in range(8):
            R.tr(PSB[4][:, g * 128:(g + 1) * 128], ws16[:, g, :], ident16)
        R.cp("dve", WST, PSB[4][:, 0:1024].rearrange("p (g q) -> p g q", q=128))
        sgn_bc = A.alloc([512], F32)
        R.dma("sp", sgn_bc, sg_norm_g.ap().partition_broadcast(128))
        bs = A.alloc([8], F32)
        R.dma("sp", bs, sg_b.ap().rearrange("g p -> p g"), allow_slow_non_contiguous=True)
        bsbc = A.alloc([8, 64], F32)
        R.cp("dve", bsbc, bs.rearrange("p (g o) -> p g o", o=1).broadcast_to([128, 8, 64]))
        A.rel(ws32); A.rel(ws16)
        u_t = [A.alloc([512], F32) for _ in range(2)]
        g_t = [A.alloc([8, 64], F32) for _ in range(2)]
        gn_t = [A.alloc([512], BF16) for _ in range(2)]
        st_t = [A.alloc([32], F32) for _ in range(2)]
        for t in range(18):
            col = t * 128 if t < 2 else 512 + (t - 2) * 128
            pu, pg, pm = PS[0 + 2 * (t % 2)], PS[1 + 2 * (t % 2)], PS[6 + (t % 2)]
            for k in range(8):
                R.mm(pu[:, :], HT0[:, k, col:col + 128], W[:, k, 0:512], start=(k == 0), stop=(k == 7))
            for k in range(8):
                R.mm(pg[:, :], HT0[:, k, col:col + 128], W[:, k, 512:1024], start=(k == 0), stop=(k == 7))
            u = u_t[t % 2]; g = g_t[t % 2]; gn = gn_t[t % 2]; st = st_t[t % 2]
            g2 = g.rearrange("p g d -> p (g d)")
            R.act(u, pu[:, :], AF.Gelu_apprx_tanh)
            R.act(g2, pg[:, :], AF.Gelu_apprx_tanh)
            mean = st[:, 0:8]; var = st[:, 8:16]; rstd = st[:, 16:24]
            R.red(mean, g, ALU.add)
            R.ts("dve", mean, mean, 1.0 / 64, None, ALU.mult)
            R.tt("dve", g, g, mean.rearrange("p (g o) -> p g o", o=1).broadcast_to([128, 8, 64]), ALU.subtract)
            sq = A.alloc([8, 64], F32)
            R.tt("pool", sq, g, g, ALU.mult)
            R.red(var, sq, ALU.add)
            A.rel(sq)
            R.act(rstd, var, AF.Sqrt, scale=1.0 / 64, bias=EPS)
            R.recip(rstd, rstd)
            R.tt("dve", g, g, rstd.rearrange("p (g o) -> p g o", o=1).broadcast_to([128, 8, 64]), ALU.mult)
            R.tt("pool", gn, g2, sgn_bc, ALU.mult)
            for gi in range(8):
                R.mm(pm[:, gi * 64:(gi + 1) * 64], WST[:, gi, :], gn[:, gi * 64:(gi + 1) * 64])
            R.tt("dve", g2, pm[:, :], bsbc.rearrange("p g d -> p (g d)"), ALU.add)
            R.tt("dve", MIX[:, t, 512:1024], g2, u, ALU.mult)
        for tl in (u_t, g_t, gn_t, st_t):
            for x in tl:
                A.rel(x)
        A.rel(W); A.rel(WST); A.rel(sgn_bc); A.rel(bs); A.rel(bsbc)
        A.rel(HT0)
        tap("MIXb", MIX, [18, 1024])
        dbg_cc("D")
        if stop_after == "D":
            R.emit()
            return nc, tap_out

        def pair_cfg(p):
            if p == 0:
                return list(range(0, 6)), 0
            if p == 1:
                return list(range(1, 6)), 6
            if p == 14:
                return list(range(14, 19)), 16
            if p == 15:
                return list(range(14, 20)), 21
            return list(range(p, p + 5)), 11

        tabs = [A.alloc([27, 128], F32, "tab") for _ in range(2)]
        Ts = [A.alloc([6, 128], F32) for _ in range(2)]
        Ps = [A.alloc([8, 128], BF16) for _ in range(2)]
        rd = A.alloc([16], F32)
        it = 0
        stg1 = [A.alloc([8, 512], F32) for _ in range(2)] if FUSED else None
        R.dma("sp", tabs[0], nabias.ap()[0])
        for h in range(8):
            tab = tabs[h % 2]
            if h < 7:
                R.dma("sp", tabs[(h + 1) % 2], nabias.ap()[h + 1])
            if FUSED:
                for cb in range(h * 12 // 8, (h + 1) * 12 // 8):
                    ada_cb(1, cb, stg1[cb % 2], PS[7])
            j, po = h // 2, (h % 2) * 64
            for p in range(-2, 16):
                if p < 0:
                    chunks, slot0 = [], 0
                    qcol = (p + 2) * 128
                    mixt = p + 2
                else:
                    chunks, slot0 = pair_cfg(p)
                    qcol = 512 + 128 * p
                    mixt = 2 + p
                nw = len(chunks)
                T = Ts[it % 2]; Pp = Ps[it % 2]
                psa, psb = PS[(it % 2) * 2], PS[(it % 2) * 2 + 1]
                po_ps = PS[4 + (it % 2)]
                it += 1
                q = QT[po:po + 64, j, qcol:qcol + 128]
                keycols = [256 + 128 * m for m in chunks] + [0, 128]
                for s, kc in enumerate(keycols):
                    pb = psa if s < 4 else psb
                    R.mm(pb[:, (s % 4) * 128:(s % 4 + 1) * 128], KT[po:po + 64, j, kc:kc + 128], q)
                if nw:
                    n1 = min(nw, 4)
                    R.tt("dve", T[:, 0:n1, :], psa[:, 0:n1 * 128].rearrange("p (s q) -> p s q", q=128),
                         tab[:, slot0:slot0 + n1, :], ALU.add)
                    if nw > 4:
                        R.tt("dve", T[:, 4:nw, :], psb[:, 0:(nw - 4) * 128].rearrange("p (s q) -> p s q", q=128),
                             tab[:, slot0 + 4:slot0 + nw, :], ALU.add)
                    R.act(Pp[:, 0:nw, :], T[:, 0:nw, :], AF.Exp)
                for s in range(nw, nw + 2):
                    pb = psa if s < 4 else psb
                    R.act(Pp[:, s, :], pb[:, (s % 4) * 128:(s % 4 + 1) * 128], AF.Exp)
                vt = [2 + m for m in chunks] + [0, 1]
                for s, t in enumerate(vt):
                    R.mm(po_ps[:, 0:65], Pp[:, s, :], VA[:, t, h * 65:(h + 1) * 65], start=(s == 0), stop=(s == nw + 1))
                rc = rd[:, (it % 16):(it % 16) + 1]
                R.recip(rc, po_ps[:, 64:65])
                R.ts("dve", MIX[:, mixt, h * 64:(h + 1) * 64], po_ps[:, 0:64], rc, None, ALU.mult)
        if FUSED:
            ada_finish(1, PS[7])
            A.rel(stg1[0]); A.rel(stg1[1]); A.rel(condT)
        for x in tabs + Ts + Ps + [rd]:
            A.rel(x)
        A.rel(QT); A.rel(KT); A.rel(VA)
        tap("MIX", MIX, [18, 1024])
        dbg_cc("E")
        if stop_after == "E":
            R.emit()
            return nc, tap_out

        XC, XO, Xt, gb, tmp = alloc_common()
        for t in range(2):
            R.dma("sp", XC[:, t, :], ctx.ap()[t * 128:(t + 1) * 128, :])
        for t in range(16):
            R.dma("sp", XO[:, t, :], xh.ap()[256 + t * 128:256 + (t + 1) * 128, :])
        gate_bc(gb[0], 0, 2, 0)
        gate_bc(gb[1], 0, 2, 1)
        WO = A.alloc([8, 1024], BF16, "WO")
        R.dma("pool", WO, even_w_out.ap().rearrange("(k p) c -> p k c", p=128))
        MTs = [A.alloc([8, 128], BF16) for _ in range(2)]

        def proj_residual(t, MT, Wt, gbc, nk):
            for nh in range(2):
                pb = PS[nh + 2 * (t % 2)]
                for k in range(nk):
                    R.mm(pb[:, :], MT[:, k, :], Wt[:, k, nh * 512:(nh + 1) * 512], start=(k == 0), stop=(k == nk - 1))
                tm = tmp[t % 2][:, nh * 512:(nh + 1) * 512]
                R.tt("dve", tm, pb[:, :], gbc[:, nh * 512:(nh + 1) * 512], ALU.mult)
                R.tt("pool", Xt(t)[:, nh * 512:(nh + 1) * 512], Xt(t)[:, nh * 512:(nh + 1) * 512], tm, ALU.add)

        for t in range(18):
            MT = MTs[t % 2]
            pt = PSB[4 + (t % 2)]
            for k in range(8):
                R.tr(pt[:, k * 128:(k + 1) * 128], MIX[:, t, k * 128:(k + 1) * 128], ident16)
            R.cp("act", MT, pt[:, 0:1024].rearrange("p (k q) -> p k q", q=128))
            proj_residual(t, MT, WO, gb[1] if t < 2 else gb[0], 8)
        A.rel(WO); A.rel(MIX)
        for x in MTs:
            A.rel(x)
        tap("X1c", XC, [2, 1024]); tap("X1", XO, [16, 1024])
        dbg_cc("F")
        if stop_after == "F":
            R.emit()
            return nc, tap_out

        HTf = A.alloc([8, 2304], BF16, "HTf")
        norm_group([Xt(0), Xt(1)], AB[0][2], AB[0][3], 1, HTf, 0)
        for g in range(4):
            norm_group([Xt(2 + g * 4 + i) for i in range(4)], AB[0][2], AB[0][3], 0, HTf, 256 + g * 512)
        gate_bc(gb[0], 0, 5, 0)
        gate_bc(gb[1], 0, 5, 1)
        ffn(HTf, 2304, [(t, t * 128, 1 if t < 2 else 0) for t in range(18)],
            ffn_w_gate.ap(), ffn_w_up.ap(), ffn_w_down.ap(), 22, gb)
        A.rel(HTf)
        tap("X2c", XC, [2, 1024]); tap("X2", XO, [16, 1024])
        dbg_cc("G")
        if stop_after == "G":
            R.emit()
            return nc, tap_out

        if which == "l0":
            for t_ in range(2):
                R.dma("sp", c2_d.ap()[t_ * 128:(t_ + 1) * 128, :], XC[:, t_, :])
            for t_ in range(16):
                R.dma("sp", x2_d.ap()[t_ * 128:(t_ + 1) * 128, :], XO[:, t_, :])
            R.emit()
            return nc, tap_out

    if not FUSED:
        XC, XO, Xt, gb, tmp = alloc_common()
        A.rel(XC)
        for t_ in range(16):
            R.dma("sp", XO[:, t_, :], xo.ap()[t_ * 128:(t_ + 1) * 128, :])
    W1 = A.alloc([8, 672], BF16, "W1")
    W1s = A.alloc([8, 32], BF16, "W1s")
    R.dma("pool", W1, mla_w_in.ap().rearrange("(k p) c -> p k c", p=128))
    R.dma("pool", W1s, mla_w_kr_sw.ap().rearrange("(k p) c -> p k c", p=128))
    HT1 = A.alloc([8, 2048], BF16, "HT1")
    for g in range(4):
        norm_group([XO[:, g * 4 + i, :] for i in range(4)], AB[1][0], AB[1][1], 0, HT1, g * 512)
    tap("HT1", HT1, [8, 2048])
    if stop_after == "Hn":
        R.emit()
        return nc, tap_out
    sq = [A.alloc([512], BF16) for _ in range(2)]
    rst = A.alloc([512], F32)
    t1 = A.alloc([512], F32)
    t2 = A.alloc([512], F32)
    CQ = A.alloc([3, 2048], BF16, "CQ")
    CR = A.alloc([2048], F32, "CR")
    SR = A.alloc([2048], F32, "SR")
    R.dma("sp", CR[0:96, :], ropeq.ap()[0])
    R.dma("sp", SR[0:96, :], ropeq.ap()[1])
    sq3 = A.alloc([512], BF16)
    import os
    QSL = int(os.environ.get("QS", "4"))
    for b in range(4):
        c0 = 512 * b
        pc = [PS[0], PS[1], PS[2]]
        pss = PS[3]
        sqs = [sq[0], sq[1], sq3]
        for r in range(3):
            for k in range(8):
                R.mm(pc[r][:, :], W1[:, k, r * 128:(r + 1) * 128], HT1[:, k, c0:c0 + 512], start=(k == 0), stop=(k == 7))
            if QSL >= 2:
                R.act(sqs[r], pc[r][:, :], AF.Square)
            R.cp("dve", CQ[:, r, b * 512:(b + 1) * 512], pc[r][:, :])
        if QSL >= 3:
            for r in range(3):
                R.mm(pss[:, :], ones16, sqs[r], start=(r == 0), stop=(r == 2))
            R.act(rst, pss[:, :], AF.Sqrt, scale=1.0 / 384, bias=EPS)
            R.recip(rst, rst)
        if QSL >= 4:
            R.tt("dve", CR[0:96, b * 512:(b + 1) * 512], CR[0:96, b * 512:(b + 1) * 512], rst[0:96, :], ALU.mult)
            R.tt("pool", SR[0:96, b * 512:(b + 1) * 512], SR[0:96, b * 512:(b + 1) * 512], rst[0:96, :], ALU.mult)
    A.rel(sq3)
    A.rel(HT1)
    tap("CQ", CQ, [3, 2048])
    if stop_after == "Hq":
        R.emit()
        return nc, tap_out

    QD = R.dram("QD", [16, 96, 2048], BF16)
    KD = R.dram("KD", [16, 64, 8448], BF16)
    VD = R.dram("VD", [16, 128, 66 * 64], BF16)
    qg = A.alloc([3], F32)
    kvg = A.alloc([2], F32)
    R.dma("sp", qg, mla_q_norm_g.ap().rearrange("(r p) -> p r", p=128), allow_slow_non_contiguous=True)
    R.dma("sp", kvg, mla_kv_norm_g.ap().rearrange("(r p) -> p r", p=128), allow_slow_non_contiguous=True)
    WQ = A.alloc([3, 1536], BF16, "WQ")
    WQS = A.alloc([3, 1536], BF16, "WQS")
    R.dma("pool", WQ, mla_w_uq.ap().rearrange("(r p) c -> p r c", p=128))
    R.dma("pool", WQS, mla_w_uq_sw.ap().rearrange("(r p) c -> p r c", p=128))
    for r in range(3):
        R.ts("dve", WQ[:, r, :], WQ[:, r, :], qg[:, r:r + 1], None, ALU.mult)
        R.ts("pool", WQS[:, r, :], WQS[:, r, :], qg[:, r:r + 1], None, ALU.mult)
    QS = [A.alloc([2048], BF16) for _ in range(2)]
    ev = 0
    for h in range(16):
        qs = QS[h % 2]
        for b in range(4):
            pq, pqs = PS[(ev % 2) * 2], PS[(ev % 2) * 2 + 1]
            ev += 1
            for r in range(3):
                R.mm(pq[0:96, :], WQ[:, r, h * 96:(h + 1) * 96], CQ[:, r, b * 512:(b + 1) * 512], start=(r == 0), stop=(r == 2))
            for r in range(3):
                R.mm(pqs[0:96, :], WQS[:, r, h * 96:(h + 1) * 96], CQ[:, r, b * 512:(b + 1) * 512], start=(r == 0), stop=(r == 2))
            R.tt("dve", t1[0:96, :], pq[0:96, :], CR[0:96, b * 512:(b + 1) * 512], ALU.mult)
            R.tt("dve", t2[0:96, :], pqs[0:96, :], SR[0:96, b * 512:(b + 1) * 512], ALU.mult)
            R.tt("pool", qs[0:96, b * 512:(b + 1) * 512], t1[0:96, :], t2[0:96, :], ALU.add)
        R.dma("sp", QD.ap()[h], qs[0:96, :])
    for x in QS + [WQ, WQS, CQ, CR, SR, qg, t1, t2, sq[0], sq[1], rst]:
        A.rel(x)

    if stop_after == "HQ":
        R.emit()
        return nc, tap_out
    CKA = A.alloc([2, 8448], BF16, "CKA")
    KRA = A.alloc([8448], BF16, "KRA")
    HTq = A.alloc([8, 512], BF16, "HTq")
    CKt = A.alloc([512], F32, "CK")
    SKt = A.alloc([512], F32, "SK")
    sq = [A.alloc([512], BF16) for _ in range(2)]
    rst = A.alloc([512], F32)
    t1 = A.alloc([512], F32)
    t2 = A.alloc([512], F32)
    xt = [A.alloc([1024], F32) for _ in range(4)] if not FUSED else []
    for u in range(5 if FUSED else 17):
        if u == 0:
            cn, dst0 = 256, 0
            if FUSED:
                norm_group([XC[:, 0, :], XC[:, 1, :]], AB[1][0], AB[1][1], 1, HTq, 0)
                A.rel(XC)
            else:
                for i in range(2):
                    R.dma("sp", xt[i], ctx.ap()[i * 128:(i + 1) * 128, :])
                norm_group(xt[0:2], AB[1][0], AB[1][1], 1, HTq, 0)
        else:
            cn, dst0 = 512, 256 + 512 * (u - 1)
            tok0 = 512 * (u - 1)
            R.dma("sp", CKt[0:32, :], ropek.ap()[0][:, tok0:tok0 + 512])
            R.dma("sp", SKt[0:32, :], ropek.ap()[1][:, tok0:tok0 + 512])
            if FUSED:
                norm_group([XO[:, 4 * (u - 1) + i, :] for i in range(4)], AB[1][0], AB[1][1], 0, HTq, 0)
            else:
                for i in range(4):
                    R.dma("sp", xt[i], xb.ap()[tok0 + i * 128:tok0 + (i + 1) * 128, :])
                norm_group(xt, AB[1][0], AB[1][1], 0, HTq, 0)
        pc = [PS[0], PS[1]]
        pss, pk, pks = PS[2], PS[3], PS[6]
        for r in range(2):
            for k in range(8):
                R.mm(pc[r][:, 0:cn], W1[:, k, 384 + r * 128:384 + (r + 1) * 128], HTq[:, k, 0:cn],
                     start=(k == 0), stop=(k == 7))
            R.act(sq[r][:, 0:cn], pc[r][:, 0:cn], AF.Square)
        for r in range(2):
            R.mm(pss[:, 0:cn], ones16, sq[r][:, 0:cn], start=(r == 0), stop=(r == 1))
        R.act(rst[:, 0:cn], pss[:, 0:cn], AF.Sqrt, scale=1.0 / 256, bias=EPS)
        R.recip(rst[:, 0:cn], rst[:, 0:cn])
        for r in range(2):
            R.tt("dve", CKA[:, r, dst0:dst0 + cn], pc[r][:, 0:cn], rst[:, 0:cn], ALU.mult)
        for k in range(8):
            R.mm(pk[0:32, 0:cn], W1[:, k, 640:672], HTq[:, k, 0:cn], start=(k == 0), stop=(k == 7))
        if u == 0:
            R.cp("act", KRA[0:32, 0:cn], pk[0:32, 0:cn])
        else:
            for k in range(8):
                R.mm(pks[0:32, 0:cn], W1s[:, k, :], HTq[:, k, 0:cn], start=(k == 0), stop=(k == 7))
            R.tt("dve", t1[0:32, 0:cn], pk[0:32, 0:cn], CKt[0:32, 0:cn], ALU.mult)
            R.tt("dve", t2[0:32, 0:cn], pks[0:32, 0:cn], SKt[0:32, 0:cn], ALU.mult)
            R.tt("pool", KRA[0:32, dst0:dst0 + cn], t1[0:32, 0:cn], t2[0:32, 0:cn], ALU.add)
    for x in xt + sq + [rst, t1, t2, CKt, SKt, HTq, W1, W1s]:
        A.rel(x)
    if FUSED:
        kvsrc = R.dram("kvsrc", [256, 2048], BF16)
        kvdst = R.dram("kvdst", [4 * 256, 2048], BF16)
        krsrc = R.dram("krsrc", [32, 2048], BF16)
        krdst = R.dram("krdst", [4 * 32, 2048], BF16)
        R.dma("sp", kvsrc.ap().rearrange("(r p) c -> p r c", p=128), CKA[:, :, 256:2304])
        R.dma("sp", krsrc.ap(), KRA[0:32, 256:2304])
        for i_, (s_, d_) in enumerate(((kvsrc, kvdst), (krsrc, krdst))):
            R.op("pool", lambda e, s_=s_, d_=d_: e.collective_compute(
                "AllGather", ALU.bypass, replica_groups=[[0, 1, 2, 3], [4, 5, 6, 7]], ins=[s_.ap()], outs=[d_.ap()]),
                [s_.ap()], [d_.ap()], dom="cc%d" % i_, step=1)
        for rk in range(4):
            R.dma("sp", CKA[:, :, 256 + 2048 * rk:256 + 2048 * (rk + 1)],
                  kvdst.ap()[256 * rk:256 * rk + 256, :].rearrange("(r p) c -> p r c", p=128))
            R.dma("sp", KRA[0:32, 256 + 2048 * rk:256 + 2048 * (rk + 1)], krdst.ap()[32 * rk:32 * rk + 32, :])
    tap("CKA", CKA, [2, 8448]); tap("KRA", KRA, [8448])
    if stop_after == "Hkv":
        R.emit()
        return nc, tap_out
    WKV = A.alloc([2, 2048], BF16, "WKV")
    R.dma("pool", WKV, mla_w_ukv.ap().rearrange("(r p) c -> p r c", p=128))
    for r in range(2):
        R.ts("dve", WKV[:, r, :], WKV[:, r, :], kvg[:, r:r + 1], None, ALU.mult)
    KS = A.alloc([8448], BF16, "KS")
    kblocks = [(512 * b, 512) for b in range(16)] + [(8192, 256)]
    ev = 0
    for h in range(16):
        for (c0, cn) in kblocks:
            pb = PS[ev % 4]
            ev += 1
            for r in range(2):
                R.mm(pb[0:64, 0:cn], WKV[:, r, h * 128:h * 128 + 64], CKA[:, r, c0:c0 + cn], start=(r == 0), stop=(r == 1))
            if ev % 2 == 0:
                R.cp("dve", KS[0:64, c0:c0 + cn], pb[0:64, 0:cn])
            else:
                R.cp("act", KS[0:64, c0:c0 + cn], pb[0:64, 0:cn])
        R.dma("sp", KD.ap()[h], KS[0:64, :])
    A.rel(KS)
    if stop_after == "HK":
        R.emit()
        return nc, tap_out
    VS = [A.alloc([4, 66 * 64], BF16)]
    WKVv = WKV.rearrange("p r (h t d) -> p r h t d", t=2, d=64)
    for hg in range(4):
        vs = VS[0].rearrange("p h (c d) -> p h c d", d=64)
        for c in range(66):
            pb = PS[4 + (ev % 4)]
            ev += 1
            for r in range(2):
                R.mm(pb[:, 0:256].rearrange("p (h d) -> p h d", d=64), CKA[:, r, c * 128:(c + 1) * 128],
                     WKVv[:, r, 4 * hg:4 * hg + 4, 1, :], start=(r == 0), stop=(r == 1))
            if ev % 2 == 0:
                R.cp("dve", vs[:, :, c, :], pb[:, 0:256].rearrange("p (h d) -> p h d", d=64))
            else:
                R.cp("act", vs[:, :, c, :], pb[:, 0:256].rearrange("p (h d) -> p h d", d=64))
        for hh in range(4):
            R.dma("sp", VD.ap()[4 * hg + hh], VS[0][:, hh, :])
    for x in VS + [WKV, CKA, kvg]:
        A.rel(x)
    if stop_after == "H":
        R.emit()
        return nc, tap_out

    KH = [A.alloc([8448], BF16, "KH") for _ in range(2)]
    for kb in KH:
        R.dma("sp", kb[64:96, :], KRA[0:32, :])
    A.rel(KRA)
    VH0 = A.alloc([66, 65], BF16, "VH0")
    VH1 = A.alloc([66, 128], BF16, "VH1")
    QH = [A.alloc([2048], BF16) for _ in range(2)]
    PT = [A.alloc([1024], BF16) for _ in range(3)]
    OTP = [A.alloc([1, 2048], BF16) for _ in range(2)]
    WO1 = A.alloc([8, 1024], BF16, "WO1")
    rd_sb = A.alloc([512], F32)
    bc_sb = A.alloc([512], F32)
    rd_hi = A.alloc([512], BF16)
    rd_lo = A.alloc([512], BF16)
    VST = A.alloc([66, 64], BF16, "VST")
    R.dma("pool", WO1, mla_w_out.ap().rearrange("(k p) c -> p k c", p=128))
    gate_bc(gb[0], 1, 2, 0)
    R.memset("pool", VH0, 1.0)
    R.memset("pool", VH1, 0.0)
    R.memset("pool", VH1[:, :, 0:1], 1.0)
    it = 0
    for h in range(16):
        kh, qh = KH[h % 2], QH[h % 2]
        odd = h % 2
        R.dma("sp", kh[0:64, :], KD.ap()[h])
        R.dma("sp", qh[0:96, :], QD.ap()[h])
        if odd:
            vh = VH1
            R.dma("sp", VST, VD.ap()[h])
            R.cp("pool", VH1[:, :, 64:128], VST)
            M, prow, r0, r1 = 128, 0, 64, 128
        else:
            vh = VH0
            R.dma("sp", VST, VD.ap()[h])
            R.cp("pool", VH0[:, :, 0:64], VST)
            M, prow, r0, r1 = 65, 64, 0, 64
        otp = OTP[(h // 2) % 2]
        for b in range(4):
            po = PS[6]
            it += 1
            q = qh[0:96, b * 512:(b + 1) * 512]
            for cp_ in range(33 + 2):
                if cp_ < 33:
                    T = PSP[cp_ % 3]
                    for j_ in range(2):
                        c = 2 * cp_ + j_
                        R.mm(T[:, j_ * 512:(j_ + 1) * 512], kh[0:96, c * 128:(c + 1) * 128], q)
                    R.act(PT[cp_ % 3], T[:, :], AF.Exp)
                if cp_ >= 2:
                    pp = cp_ - 2
                    for j_ in range(2):
                        cc = 2 * pp + j_
                        R.mm(po[0:M, :], vh[:, cc, 0:M], PT[pp % 3][:, j_ * 512:(j_ + 1) * 512],
                             start=(cc == 0), stop=(cc == 65))
            R.recip(rd_sb[prow:prow + 1, :], po[prow:prow + 1, :])
            R.cp("dve", rd_hi[prow:prow + 1, :], rd_sb[prow:prow + 1, :])
            R.tt("dve", rd_sb[prow:prow + 1, :], rd_sb[prow:prow + 1, :], rd_hi[prow:prow + 1, :], ALU.subtract)
            R.cp("dve", rd_lo[prow:prow + 1, :], rd_sb[prow:prow + 1, :])
            nb_ = 128 if odd else 64
            R.mm(PS[7][0:nb_, :], ones16[prow:prow + 1, 0:nb_], rd_hi[prow:prow + 1, :], start=True, stop=False)
            R.mm(PS[7][0:nb_, :], ones16[prow:prow + 1, 0:nb_], rd_lo[prow:prow + 1, :], start=False, stop=True)
            R.cp("act", bc_sb[r0:r1, :], PS[7][r0:r1, :])
            R.tt("dve", otp[r0:r1, 0, b * 512:(b + 1) * 512], po[r0:r1, :], bc_sb[r0:r1, :], ALU.mult)
        if odd:
            j = h // 2
            for t in range(2, 18):
                for nh in range(2):
                    pb = PS[6 + nh]
                    R.mm(pb[:, :], otp[:, 0, (t - 2) * 128:(t - 1) * 128], WO1[:, j, nh * 512:(nh + 1) * 512])
                    tm = tmp[t % 2][:, nh * 512:(nh + 1) * 512]
                    xs = Xt(t)[:, nh * 512:(nh + 1) * 512]
                    R.tt("dve", tm, pb[:, :], gb[0][:, nh * 512:(nh + 1) * 512], ALU.mult)
                    R.tt("pool", xs, xs, tm, ALU.add)
    for x in KH + QH + PT + OTP + [VH0, VH1, WO1, rd_sb, bc_sb, VST, rd_hi, rd_lo]:
        A.rel(x)
    tap("X3", XO, [16, 1024])
    if stop_after == "I":
        R.emit()
        return nc, tap_out

    HT2 = A.alloc([8, 2048], BF16, "HT2")
    WR = A.alloc([8, 8], F32)
    R.dma("sp", WR, moe_w_router.ap().rearrange("(k p) e -> p k e", p=128))
    brbc = A.alloc([8], F32)
    R.dma("sp", brbc, moe_b_router.ap().partition_broadcast(128))
    COMB = A.alloc([16, 8], F32, "COMB")
    xn32 = A.alloc([1024], F32)
    hT32 = A.alloc([8, 128], F32)
    junk = A.alloc([1024], BF16)
    st = A.alloc([16], F32)
    lg = A.alloc([8], F32)
    lg2 = A.alloc([8], F32)
    eq1 = A.alloc([8], F32)
    eq2 = A.alloc([8], F32)
    Af, Bf = AB[1][2], AB[1][3]
    for t in range(16):
        x = XO[:, t, :]
        ss, rs, m1, m2, dd, ee, w1, w2 = [st[:, i:i + 1] for i in range(8)]
        R.act(junk, x, AF.Square, accum_out=ss)
        R.act(rs, ss, AF.Sqrt, scale=1.0 / 1024, bias=EPS)
        R.recip(rs, rs)
        R.ts("dve", xn32, x, rs, None, ALU.mult)
        for k in range(8):
            pb = PS[k // 4]
            R.tr(pb[:, (k % 4) * 128:(k % 4 + 1) * 128], xn32[:, k * 128:(k + 1) * 128], ident32)
        for k in range(8):
            pb = PS[k // 4]
            src = pb[:, (k % 4) * 128:(k % 4 + 1) * 128]
            if k % 2 == 0:
                R.ts("dve", hT32[:, k, :], src, Af[:, k, 0:1], Bf[:, k, 0:1], ALU.mult, ALU.add)
            else:
                R.act(hT32[:, k, :], src, AF.Identity, scale=Af[:, k, 0:1], bias=Bf[:, k, 0:1])
        R.cp("pool", HT2[:, :, t * 128:(t + 1) * 128], hT32)
        pl = PS[2]
        for k in range(8):
            R.mm(pl[:, 0:8], hT32[:, k, :], WR[:, k, :], start=(k == 0), stop=(k == 7))
        R.tt("dve", lg, pl[:, 0:8], brbc, ALU.add)
        R.red(m1, lg, ALU.max)
        R.ts("dve", eq1, lg, m1, None, ALU.is_equal)
        R.stt("dve", lg2, eq1, -1e30, lg, ALU.mult, ALU.add)
        R.red(m2, lg2, ALU.max)
        R.ts("dve", eq2, lg2, m2, None, ALU.is_equal)
        R.tt("dve", dd, m2, m1, ALU.subtract)
        R.act(ee, dd, AF.Exp)
        R.ts("dve", w1, ee, 1.0, None, ALU.add)
        R.recip(w1, w1)
        R.tt("dve", w2, ee, w1, ALU.mult)
        R.ts("dve", eq1, eq1, w1, None, ALU.mult)
        R.stt("dve", COMB[:, t, :], eq2, w2, eq1, ALU.mult, ALU.add)
    for x in (xn32, hT32, junk, lg, lg2, eq1, eq2, WR, brbc):
        A.rel(x)
    tap("COMB", COMB, [16, 8])
    gate_bc(gb[0], 1, 5, 0)
    ne = 8 if stop_after != "J1" else 1
    for e in range(ne):
        ffn(HT2, 2048, [(t, (t - 2) * 128, 0) for t in range(2, 18)],
            moe_w_gate.ap()[e], moe_w_up.ap()[e], moe_w_down.ap()[e], 28, gb,
            comb=lambda t, e=e: COMB[:, t - 2, e:e + 1])
    A.rel(HT2)

    fgbc = A.alloc([1024], F32)
    R.dma("sp", fgbc, final_g.ap().partition_broadcast(128))
    ot = [A.alloc([1024], F32) for _ in range(2)]
    junk = A.alloc([1024], BF16)
    for t in range(16):
        x = XO[:, t, :]
        ss, rs = st[:, 8:9], st[:, 9:10]
        R.act(junk, x, AF.Square, accum_out=ss)
        R.act(rs, ss, AF.Sqrt, scale=1.0 / 1024, bias=EPS)
        R.recip(rs, rs)
        R.stt("dve", ot[t % 2], x, rs, fgbc, ALU.mult, ALU.mult)
        R.dma("sp", out_d.ap()[t * 128:(t + 1) * 128, :], ot[t % 2])
    R.emit()
    return nc, tap_out


def _na_bias_tables(rpb, cpos):
    NEG = np.float32(-30000.0)
    tab = np.full((8, 27, 128, 128), NEG, np.float32)
    rows_b = 128

    def cfg(p):
        if p == 0:
            return list(range(0, 6)), 0
        if p == 1:
            return list(range(1, 6)), 6
        if p == 14:
            return list(range(14, 19)), 16
        if p == 15:
            return list(range(14, 20)), 21
        return list(range(p, p + 5)), 11

    cols = np.arange(64)
    cstart = np.clip(cols - 8, 0, 48)
    for p in (0, 1, 2, 14, 15):
        chunks, slot0 = cfg(p)
        for s, m in enumerate(chunks):
            for kr_l in range(2):
                jrow = 2 * m + kr_l
                grow = 32 * cpos - 4 + jrow
                for qr_l in range(2):
                    i = 2 * p + qr_l
                    r = 32 * cpos + i
                    rs = min(max(r - 4, 0), rows_b - 8)
                    if grow < rs or grow >= rs + 8 or grow < 0 or grow >= rows_b:
                        continue
                    ridx = grow - r + 7
                    kc = cols[:, None]
                    qc = cols[None, :]
                    valid = (kc >= cstart[None, :]) & (kc < cstart[None, :] + 16)
                    cidx = np.clip(kc - qc + 15, 0, 30)
                    vals = rpb[:, ridx, :][:, cidx]
                    blk = np.where(valid[None], vals, NEG)
                    tab[:, slot0 + s, kr_l * 64:(kr_l + 1) * 64, qr_l * 64:(qr_l + 1) * 64] = blk
    return np.ascontiguousarray(tab.transpose(0, 2, 1, 3))


def _rope_tables(cpos):
    t = np.arange(2048, dtype=np.int64) + 2048 * cpos
    pos = np.stack([t // GRID_W, t % GRID_W], -1).astype(np.float32)
    inv = np.power(np.float32(10000.0), -np.arange(8, dtype=np.float32) / np.float32(8)).astype(np.float32)
    ang = pos[:, :, None] * inv
    cos = np.cos(ang).astype(np.float32)
    sin = np.sin(ang).astype(np.float32)
    C = np.zeros((32, 2048), np.float32)
    S = np.zeros((32, 2048), np.float32)
    for a in range(2):
        for half in range(2):
            rows = slice(a * 16 + half * 8, a * 16 + half * 8 + 8)
            C[rows] = cos[:, a, :].T
            S[rows] = (-sin[:, a, :].T) if half == 0 else sin[:, a, :].T
    scale = np.float32(96.0 ** -0.5)
    Cq = np.concatenate([np.full((64, 2048), scale, np.float32), C * scale], 0)
    Sq = np.concatenate([np.zeros((64, 2048), np.float32), S * scale], 0)
    return np.stack([Cq, Sq]), np.stack([C, S])


_SWAP32 = np.concatenate([np.arange(8, 16), np.arange(0, 8), np.arange(24, 32), np.arange(16, 24)])


def _rope_k_full():
    t = np.arange(8192, dtype=np.int64)
    pos = np.stack([t // GRID_W, t % GRID_W], -1).astype(np.float32)
    inv = np.power(np.float32(10000.0), -np.arange(8, dtype=np.float32) / np.float32(8)).astype(np.float32)
    ang = pos[:, :, None] * inv
    cos = np.cos(ang).astype(np.float32)
    sin = np.sin(ang).astype(np.float32)
    C = np.zeros((32, 8192), np.float32)
    S = np.zeros((32, 8192), np.float32)
    for a in range(2):
        for half in range(2):
            rows = slice(a * 16 + half * 8, a * 16 + half * 8 + 8)
            C[rows] = cos[:, a, :].T
            S[rows] = (-sin[:, a, :].T) if half == 0 else sin[:, a, :].T
    return np.stack([C, S])


_f = lambda a: np.ascontiguousarray(np.asarray(a, dtype=np.float32))


def make_maps_l0(inp):
    f = _f
    x = f(inp["x"]); ctxa = f(inp["ctx"])
    shared = dict(
        ada_w=f(inp["ada_w"]), ada_b=f(inp["ada_b"]), norm_g=f(inp["norm_g"]),
        na_w_in=f(inp["na_w_in"][0]), sg_w=f(inp["sg_w"][0]), sg_b=f(inp["sg_b"][0]), sg_norm_g=f(inp["sg_norm_g"][0]),
        even_w_out=f(inp["even_w_out"][0]), ffn_w_gate=f(inp["ffn_w_gate"][0]), ffn_w_up=f(inp["ffn_w_up"][0]),
        ffn_w_down=f(inp["ffn_w_down"][0]), ident=np.eye(128, dtype=np.float32),
    )
    rpb = f(inp["na_rpb"][0])
    maps = []
    for c in range(8):
        b, cp = c // 4, c % 4
        xhp = np.zeros((2560, 1024), np.float32)
        lo = 2048 * cp - 256
        s0, s1 = max(lo, 0), min(lo + 2560, 8192)
        xhp[s0 - lo:s1 - lo] = x[b, s0:s1]
        m = dict(shared)
        m.update(xh=xhp, ctx=ctxa[b], cond=np.stack([f(inp["c"])[b], f(inp["c_ctx"])]),
                 nabias=_na_bias_tables(rpb, cp))
        maps.append(m)
    return maps


def make_maps_l1(inp, x2, c2, fused=False):
    f = _f
    uq = f(inp["mla_w_uq"][0])
    perm = np.arange(1536).reshape(16, 96).copy()
    perm[:, 64:] = perm[:, 64:][:, _SWAP32]
    uq_sw = np.ascontiguousarray(uq[:, perm.reshape(-1)])
    w_in1 = f(inp["mla_w_in"][0])
    kr_sw = np.ascontiguousarray(w_in1[:, 640:672][:, _SWAP32])
    shared = dict(
        ada_w=f(inp["ada_w"]), ada_b=f(inp["ada_b"]), norm_g=f(inp["norm_g"]), final_g=f(inp["final_g"]),
        mla_w_in=w_in1, mla_w_kr_sw=kr_sw,
        mla_q_norm_g=f(inp["mla_q_norm_g"][0]), mla_kv_norm_g=f(inp["mla_kv_norm_g"][0]),
        mla_w_uq=uq, mla_w_uq_sw=uq_sw, mla_w_ukv=f(inp["mla_w_ukv"][0]), mla_w_out=f(inp["mla_w_out"][0]),
        moe_w_router=f(inp["moe_w_router"][0]), moe_b_router=f(inp["moe_b_router"][0]),
        moe_w_gate=f(inp["moe_w_gate"][0]), moe_w_up=f(inp["moe_w_up"][0]), moe_w_down=f(inp["moe_w_down"][0]),
        ident=np.eye(128, dtype=np.float32), ropek=_rope_k_full(),
    )
    maps = []
    for c in range(8):
        b, cp = c // 4, c % 4
        rq, rk_ = _rope_tables(cp)
        m = dict(shared)
        if fused:
            m.update(ropek=rk_, ropeq=rq)
        else:
            m.update(xb=np.ascontiguousarray(x2[b]), xo=np.ascontiguousarray(x2[b, 2048 * cp:2048 * (cp + 1)]),
                     ctx=np.ascontiguousarray(c2[b]), cond=np.stack([f(inp["c"])[b], f(inp["c_ctx"])]), ropeq=rq)
        maps.append(m)
    return maps


def make_maps_fused(inp):
    m0 = make_maps_l0(inp)
    x = _f(inp["x"]); ctxa = _f(inp["ctx"])
    m1 = make_maps_l1(inp, x, ctxa, fused=True)
    maps = []
    for c in range(8):
        m = dict(m1[c])
        m.update(m0[c])
        maps.append(m)
    return maps


def kernel(**inputs):
    nc, _ = build("fused")
    res = run_bass_kernel_spmd(nc, make_maps_fused(inputs), core_ids=list(range(8)))
    out = np.zeros((2, 8192, 1024), np.float32)
    for c in range(8):
        b, cp = c // 4, c % 4
        out[b, 2048 * cp:2048 * (cp + 1)] = res.results[c]["out"]
    return out
```

```python
import numpy as np
import concourse.bass as bass
import concourse.mybir as mybir
from concourse.bass_utils import run_bass_kernel_spmd

F32 = mybir.dt.float32
BF16 = mybir.dt.bfloat16
AF = mybir.ActivationFunctionType
ALU = mybir.AluOpType
AX = mybir.AxisListType
DTSZ = {F32: 4, BF16: 2}
NSLOT = 24

GRID_W = 64
_NOCC = 0
_CCFIRST = None
EPS = 1e-6


class Op:
    __slots__ = ("eng", "fn", "dom", "idx", "waits", "signal", "clock", "is_dma", "cnt", "step")


class Rec:
    def __init__(self, nc):
        self.nc = nc
        self.ops = []
        self.meta = {}
        self.live = {}
        self.known = {}
        self.dom_ops = {}
        self.slot_rr = {}
        self.slot_last = {}

    def sb(self, name, shape, dt):
        t = self.nc.alloc_sbuf_tensor(name, list(shape), dt)
        self.meta[name] = ("sb", int(np.prod(shape[1:])) * DTSZ[dt])
        return t

    def ps(self, name, shape, dt=F32):
        t = self.nc.alloc_psum_tensor(name, list(shape), dt)
        self.meta[name] = ("ps", int(np.prod(shape[1:])) * DTSZ[dt])
        return t

    def dram(self, name, shape, dt, kind="Internal"):
        t = self.nc.dram_tensor(name, list(shape), dt, kind=kind)
        self.meta[name] = ("dram", 0)
        return t

    def box(self, ap):
        name = ap.tensor.name
        kind, row = self.meta[name]
        sz = DTSZ[ap.dtype]
        off = int(ap.offset) * sz
        dims = ap.ap
        if kind == "dram":
            hi = off + sum((c - 1) * abs(s) for s, c in dims) * sz + sz - 1
            return (name, 0, 0, off, hi)
        p0 = off // row
        f0 = off % row
        ps_, pc = dims[0]
        pstep = (abs(ps_) * sz) // row if pc > 1 else 0
        p1 = p0 + (pc - 1) * pstep
        f1 = f0 + sum((c - 1) * abs(s) for s, c in dims[1:]) * sz + sz - 1
        return (name, p0, p1, f0, f1)

    @staticmethod
    def _ovl(a, b):
        return a[1] <= b[2] and b[1] <= a[2] and a[3] <= b[4] and b[3] <= a[4]

    @staticmethod
    def _cov(a, b):
        return a[1] <= b[1] and a[2] >= b[2] and a[3] <= b[3] and a[4] >= b[4]

    def op(self, eng, fn, reads=(), writes=(), dma=False, dom=None, step=None):
        o = Op()
        o.eng = eng
        o.fn = fn
        o.is_dma = dma
        o.signal = False
        o.waits = {}
        E = eng
        known = self.known.setdefault(E, {})
        deps = {}
        rb = [self.box(a) for a in reads]
        wb = [self.box(a) for a in writes]
        psb = set()
        for lst_ in (rb, wb):
            for i_, b in enumerate(lst_):
                if self.meta[b[0]][0] == "ps":
                    lst_[i_] = (b[0], 0, 127, (b[3] // 2048) * 2048, (b[4] // 2048) * 2048 + 2047)
                    psb.add(b[0])
        for b in rb:
            ps_ = b[0] in psb
            for (lb, lop, lw) in self.live.get(b[0], ()):
                if self._ovl(b, lb):
                    if lw:
                        deps[lop] = True
                    elif ps_ and lop.eng != E:
                        deps[lop] = True
        for b in wb:
            for (lb, lop, lw) in self.live.get(b[0], ()):
                if self._ovl(b, lb):
                    deps[lop] = deps.get(lop, False) or lw
        if dma:
            rr = self.slot_rr.get(E, 0)
            self.slot_rr[E] = rr + 1
            dom = ("dma", E, rr % NSLOT)
            prev = self.slot_last.get(dom)
            if prev is not None:
                deps[prev] = True
            self.slot_last[dom] = o
        elif dom is None:
            dom = E
        o.dom = dom
        o.step = step if step is not None else (16 if dma else 1)
        for y, hard in deps.items():
            if y.dom == E:
                if E == "pe" or not hard:
                    continue
            if known.get(y.dom, -1) >= y.idx:
                continue
            if o.waits.get(y.dom, (-1, None))[0] < y.idx:
                o.waits[y.dom] = (y.idx, y)
        for d, (i, y) in o.waits.items():
            y.signal = True
            for k, v in y.clock.items():
                if known.get(k, -1) < v:
                    known[k] = v
        lst = self.dom_ops.setdefault(dom, [])
        o.idx = len(lst)
        lst.append(o)
        o.clock = dict(known)
        o.clock[dom] = o.idx
        for b in wb:
            l = self.live.setdefault(b[0], [])
            l[:] = [x for x in l if not self._cov(b, x[0])]
            l.append((b, o, True))
        for b in rb:
            l = self.live.setdefault(b[0], [])
            l[:] = [x for x in l if not (x[1].dom == dom and not x[2] and x[0] == b)]
            l.append((b, o, False))
        self.ops.append(o)
        return o

    def mm(self, out, lhsT, rhs, start=True, stop=True):
        return self.op("pe", lambda e: e.matmul(out, lhsT, rhs, start=start, stop=stop), [lhsT, rhs], [out])

    def tr(self, out, in_, ident):
        return self.op("pe", lambda e: e.transpose(out, in_, ident), [in_, ident], [out])

    def act(self, out, in_, func, bias=None, scale=None, accum_out=None):
        kw = {}
        rd = [in_]
        wr = [out]
        if bias is not None:
            kw["bias"] = bias
            if not isinstance(bias, (int, float)):
                rd.append(bias)
        if scale is not None:
            kw["scale"] = scale
            if not isinstance(scale, (int, float)):
                rd.append(scale)
        if accum_out is not None:
            kw["accum_out"] = accum_out
            wr.append(accum_out)
        return self.op("act", lambda e: e.activation(out, in_, func, **kw), rd, wr)

    def tt(self, eng, out, in0, in1, op):
        return self.op(eng, lambda e: e.tensor_tensor(out, in0, in1, op), [in0, in1], [out])

    def ts(self, eng, out, in0, s1, s2, op0, op1=None, accum_out=None):
        rd = [in0] + [s for s in (s1, s2) if s is not None and not isinstance(s, (int, float))]
        wr = [out] + ([accum_out] if accum_out is not None else [])
        kw = {}
        if op1 is not None:
            kw["op1"] = op1
        if accum_out is not None:
            kw["accum_out"] = accum_out
        return self.op(eng, lambda e: e.tensor_scalar(out, in0, s1, s2, op0, **kw), rd, wr)

    def stt(self, eng, out, in0, scalar, in1, op0, op1):
        rd = [in0, in1] + ([scalar] if not isinstance(scalar, (int, float)) else [])
        return self.op(eng, lambda e: e.scalar_tensor_tensor(out, in0, scalar, in1, op0, op1), rd, [out])

    def cp(self, eng, out, in_):
        if eng == "act":
            return self.op(eng, lambda e: e.copy(out, in_), [in_], [out])
        return self.op(eng, lambda e: e.tensor_copy(out, in_), [in_], [out])

    def memset(self, eng, out, val):
        return self.op(eng, lambda e: e.memset(out, val), [], [out])

    def recip(self, out, in_):
        return self.op("dve", lambda e: e.reciprocal(out, in_), [in_], [out])

    def red(self, out, in_, op):
        return self.op("dve", lambda e: e.tensor_reduce(out, in_, AX.X, op), [in_], [out])

    def dma(self, q, out, in_, **kw):
        return self.op(q, lambda e: e.dma_start(out, in_, **kw), [in_], [out], dma=True)

    def emit(self):
        nc = self.nc
        tails = {}
        for dom, last in self.slot_last.items():
            tails.setdefault(dom[1], []).append(last)
            last.signal = True
        sems = {}
        for dom, lst in self.dom_ops.items():
            nm = "s_" + ("_".join(str(x) for x in dom) if isinstance(dom, tuple) else dom)
            sems[dom] = nc.alloc_semaphore(nm)
            c = 0
            for o in lst:
                if o.signal:
                    c += o.step
                o.cnt = c
        per_eng = {}
        for o in self.ops:
            per_eng.setdefault(o.eng, []).append(o)
        engmap = {"pe": "tensor", "act": "scalar", "dve": "vector", "pool": "gpsimd", "sp": "sync"}
        with nc.Block() as block:
            for E, lst in per_eng.items():
                def body(e, lst=lst, E=E):
                    for o in lst:
                        for d, (i, y) in o.waits.items():
                            e.wait_ge(sems[d], y.cnt)
                        ins = o.fn(e)
                        if o.signal:
                            ins.then_inc(sems[o.dom], o.step)
                    for last in tails.get(E, ()):
                        e.wait_ge(sems[last.dom], last.cnt)
                getattr(block, engmap[E])(body)


class Arena:
    def __init__(self, R, cap):
        self.t = R.sb("arena", [128, cap // 2], BF16)
        self.free = [(0, cap)]
        self.used = {}

    def alloc(self, shape, dt, tag=None):
        n = int(np.prod(shape)) * DTSZ[dt]
        na = (n + 63) // 64 * 64
        for i, (o, s) in enumerate(self.free):
            if s >= na:
                self.free[i] = (o + na, s - na)
                break
        else:
            raise MemoryError(f"arena full: want {na} free {self.free} tag {tag}")
        ap = self.t[:, o // 2:(o + n) // 2]
        if dt != BF16:
            ap = ap.bitcast(dt)
        if len(shape) == 2:
            ap = ap.rearrange("p (a b) -> p a b", b=shape[1])
        elif len(shape) == 3:
            ap = ap.rearrange("p (a b c) -> p a b c", b=shape[1], c=shape[2])
        self.used[id(ap)] = (o, na)
        ap_key = ap
        self._last = (o, na)
        return ap

    def rel(self, ap):
        o, na = self.used.pop(id(ap))
        self.free.append((o, na))
        self.free.sort()
        m = []
        for o, s in self.free:
            if m and m[-1][0] + m[-1][1] == o:
                m[-1] = (m[-1][0], m[-1][1] + s)
            else:
                m.append((o, s))
        self.free = [x for x in m if x[1] > 0]


def build(which, stop_after=None, taps=()):
    nc = bass.Bass("TRN2", target_bir_lowering=False)
    R = Rec(nc)
    A = Arena(R, 206 * 1024)
    PSP = [R.ps(f"psp{i}", [128, 1024], F32) for i in range(4)]
    PS = [PSP[i // 2][:, (i % 2) * 512:(i % 2 + 1) * 512] for i in range(8)]
    PSB = [p.bitcast(BF16) for p in PS]
    tap_out = {}

    def din(name, shape):
        return R.dram(name, shape, F32, kind="ExternalInput")

    L0 = which in ("l0", "fused")
    L1 = which in ("l1", "fused")
    FUSED = which == "fused"
    d0 = lambda n, sh: din(n, sh) if L0 else None
    d1 = lambda n, sh: din(n, sh) if L1 else None
    xh = d0("xh", [2560, 1024])
    ctx = din("ctx", [256, 1024])
    cond = din("cond", [2, 1024])
    ada_w = din("ada_w", [2, 1024, 6144])
    ada_b = din("ada_b", [2, 6144])
    norm_g = din("norm_g", [2, 2, 1024])
    final_g = d1("final_g", [1024])
    na_w_in = d0("na_w_in", [1024, 2560])
    nabias = d0("nabias", [8, 128, 27, 128])
    sg_w = d0("sg_w", [8, 128, 128])
    sg_b = d0("sg_b", [8, 128])
    sg_norm_g = d0("sg_norm_g", [512])
    even_w_out = d0("even_w_out", [1024, 1024])
    ffn_w_gate = d0("ffn_w_gate", [1024, 2816])
    ffn_w_up = d0("ffn_w_up", [1024, 2816])
    ffn_w_down = d0("ffn_w_down", [2816, 1024])
    xb = din("xb", [8192, 1024]) if which == "l1" else None
    xo = din("xo", [2048, 1024]) if which == "l1" else None
    mla_w_in = d1("mla_w_in", [1024, 672])
    mla_w_kr_sw = d1("mla_w_kr_sw", [1024, 32])
    mla_q_norm_g = d1("mla_q_norm_g", [384])
    mla_kv_norm_g = d1("mla_kv_norm_g", [256])
    mla_w_uq = d1("mla_w_uq", [384, 1536])
    mla_w_uq_sw = d1("mla_w_uq_sw", [384, 1536])
    mla_w_ukv = d1("mla_w_ukv", [256, 2048])
    mla_w_out = d1("mla_w_out", [1024, 1024])
    moe_w_router = d1("moe_w_router", [1024, 8])
    moe_b_router = d1("moe_b_router", [8])
    moe_w_gate = d1("moe_w_gate", [8, 1024, 3584])
    moe_w_up = d1("moe_w_up", [8, 1024, 3584])
    moe_w_down = d1("moe_w_down", [8, 3584, 1024])
    ropeq = d1("ropeq", [2, 96, 2048])
    ropek = d1("ropek", [2, 32, 2048 if FUSED else 8192])
    ident_d = din("ident", [128, 128])
    if which == "l0":
        x2_d = R.dram("x2", [2048, 1024], F32, kind="ExternalOutput")
        c2_d = R.dram("c2", [256, 1024], F32, kind="ExternalOutput")
    else:
        out_d = R.dram("out", [2048, 1024], F32, kind="ExternalOutput")

    def tap(name, ap_sb, shape):
        if name not in taps:
            return
        t = R.dram("tap_" + name, [128] + list(shape), ap_sb.dtype, kind="ExternalOutput")
        tap_out[name] = t
        R.dma("sp", t.ap(), ap_sb)

    ident32 = A.alloc([128], F32)
    ident16 = A.alloc([128], BF16)
    ones32 = A.alloc([128], F32)
    modv = [A.alloc([48, 2], F32) for _ in range(2)]
    AB = [[A.alloc([8, 2], F32) for _ in range(4)] for _ in range(2)]
    R.dma("sp", ident32, ident_d.ap())
    R.cp("dve", ident16, ident32)
    ones16 = A.alloc([128], BF16)
    R.memset("dve", ones32, 1.0)
    R.memset("dve", ones16, 1.0)

    def dbg_cc(pos):
        if _CCFIRST != pos:
            return
        d_s = R.dram("dbg_s", [32, 2048], BF16)
        d_d = R.dram("dbg_d", [128, 2048], BF16)
        R.dma("sp", d_s.ap()[:, 0:128], ident16[0:32, :])
        R.op("pool", lambda e: e.collective_compute(
            "AllGather", ALU.bypass, replica_groups=[[0, 1, 2, 3], [4, 5, 6, 7]], ins=[d_s.ap()], outs=[d_d.ap()]),
            [d_s.ap()], [d_d.ap()], dom="ccx", step=1)
        R.dma("sp", ones32[0:32, 0:64].bitcast(BF16), d_d.ap()[32:64, 0:128])
        R.memset("dve", ones32, 1.0)

    dbg_cc("A")
    condT = A.alloc([8, 2], F32)
    for r_ in range(2):
        R.dma("sp", condT[:, :, r_], cond.ap()[r_].rearrange("(k p) -> p k", p=128), allow_slow_non_contiguous=True)
    R.act(condT, condT, AF.Silu)
    stg = [A.alloc([8, 512], F32) for _ in range(2)]
    adab = A.alloc([48], F32)
    ng = A.alloc([2, 8], F32)
    psA = PS[0]
    si = 0
    for l in ((0, 1) if FUSED else ((0,) if L0 else (1,))):
        for cb in range(12):
            wt = stg[si % 2]
            si += 1
            R.dma("sp", wt, ada_w.ap()[l, :, cb * 512:(cb + 1) * 512].rearrange("(k p) c -> p k c", p=128))
            for fc in range(4):
                j = cb * 4 + fc
                for k in range(8):
                    R.mm(psA[:, (l * 48 + j) * 2:(l * 48 + j) * 2 + 2], wt[:, k, fc * 128:(fc + 1) * 128],
                         condT[:, k, :], start=(k == 0), stop=(k == 7))
        R.dma("sp", adab, ada_b.ap()[l].rearrange("(j p) -> p j", p=128), allow_slow_non_contiguous=True)
        for w_ in range(2):
            R.dma("sp", ng[:, w_, :], norm_g.ap()[l, w_].rearrange("(k p) -> p k", p=128), allow_slow_non_contiguous=True)
        R.tt("dve", modv[l], psA[:, l * 96:(l + 1) * 96].rearrange("p (j c) -> p j c", c=2),
             adab.rearrange("p (j o) -> p j o", o=1).broadcast_to([128, 48, 2]), ALU.add)
        for w in range(2):
            sh = modv[l][:, w * 24:w * 24 + 8, :]
            sc = modv[l][:, w * 24 + 8:w * 24 + 16, :]
            Am, Bm = AB[l][2 * w], AB[l][2 * w + 1]
            R.ts("dve", Am, sc, 1.0, None, ALU.add)
            R.tt("dve", Am, Am, ng[:, w, :].rearrange("p (k o) -> p k o", o=1).broadcast_to([128, 8, 2]), ALU.mult)
            R.cp("dve", Bm, sh)
    A.rel(stg[0]); A.rel(stg[1]); A.rel(adab); A.rel(ng); A.rel(condT)
    tap("modv0", modv[0], [48, 2])
    tap("AB0", AB[0][0], [8, 2])

    def gate_bc(dst, l, which, c):
        dg = A.alloc([2, 128], F32)
        for k in range(8):
            d = dg[:, k % 2, :]
            R.ts("dve", d, ident32, modv[l][:, which * 8 + k, c:c + 1], None, ALU.mult)
            pb = PS[6 + (k // 4)]
            R.mm(pb[:, (k % 4) * 128:(k % 4 + 1) * 128], ones32, d)
        R.cp("act", dst[:, 0:512], PS[6][:, :])
        R.cp("act", dst[:, 512:1024], PS[7][:, :])
        A.rel(dg)

    def norm_group(xs, Aap, Bap, c, HT, col0, want32=None):
        n = len(xs)
        ss = A.alloc([4], F32)
        rs = A.alloc([4], F32)
        junk = A.alloc([1024], BF16)
        xn = [A.alloc([1024], BF16) for _ in range(n)]
        for i, x in enumerate(xs):
            R.act(junk, x, AF.Square, accum_out=ss[:, i:i + 1])
        R.act(rs[:, 0:n], ss[:, 0:n], AF.Sqrt, scale=1.0 / 1024, bias=EPS)
        R.recip(rs[:, 0:n], rs[:, 0:n])
        for i, x in enumerate(xs):
            R.ts("dve" if i % 2 == 0 else "pool", xn[i], x, rs[:, i:i + 1], None, ALU.mult)
        for k in range(8):
            pst = PSB[4 + (k % 2)][:, 0:n * 128]
            for i in range(n):
                R.tr(pst[:, i * 128:(i + 1) * 128], xn[i][:, k * 128:(k + 1) * 128], ident16)
            dst = HT[:, k, col0:col0 + n * 128]
            if k % 2 == 0:
                R.ts("dve", dst, pst, Aap[:, k, c:c + 1], Bap[:, k, c:c + 1], ALU.mult, ALU.add)
            else:
                R.act(dst, pst, AF.Identity, scale=Aap[:, k, c:c + 1], bias=Bap[:, k, c:c + 1])
        for t in xn:
            A.rel(t)
        A.rel(junk); A.rel(rs); A.rel(ss)

    _cm = {}

    def alloc_common():
        XC = A.alloc([2, 1024], F32, "XC")
        XO = A.alloc([16, 1024], F32, "XO")

        def Xt(t):
            return XC[:, t, :] if t < 2 else XO[:, t - 2, :]

        gb = [A.alloc([1024], F32) for _ in range(2)]
        tmp = [A.alloc([1024], F32) for _ in range(2)]
        _cm.update(XC=XC, XO=XO, Xt=Xt, gb=gb, tmp=tmp)
        return XC, XO, Xt, gb, tmp

    def ffn(HT, ncols, tiles, wg_ap, wu_ap, wd_ap, nf, gbcs, comb=None):
        GS = 4
        ngrp = (nf + GS - 1) // GS
        Wg = [A.alloc([8, GS * 128], BF16) for _ in range(2)]
        Wu = [A.alloc([8, GS * 128], BF16) for _ in range(2)]
        Wd = [A.alloc([GS, 1024], BF16) for _ in range(2)]
        AT = A.alloc([GS, ncols], BF16, "AT")
        Sg = [A.alloc([512], BF16) for _ in range(2)]
        blocks = [(b, min(512, ncols - b)) for b in range(0, ncols, 512)]
        ev = 0
        for gi in range(ngrp):
            f0 = gi * GS
            gs = min(GS, nf - f0)
            wg, wu, wd = Wg[gi % 2], Wu[gi % 2], Wd[gi % 2]
            R.dma("pool", wg[:, :, 0:gs * 128], wg_ap[:, f0 * 128:(f0 + gs) * 128].rearrange("(k p) c -> p k c", p=128))
            R.dma("pool", wu[:, :, 0:gs * 128], wu_ap[:, f0 * 128:(f0 + gs) * 128].rearrange("(k p) c -> p k c", p=128))
            R.dma("pool", wd[:, 0:gs, :], wd_ap[f0 * 128:(f0 + gs) * 128, :].rearrange("(k p) c -> p k c", p=128))
            for (c0, cn) in blocks:
                for fc in range(gs):
                    pg, pu = PS[(ev % 2) * 2], PS[(ev % 2) * 2 + 1]
                    sgt = Sg[ev % 2]
                    ev += 1
                    for k in range(8):
                        R.mm(pg[:, 0:cn], wg[:, k, fc * 128:(fc + 1) * 128], HT[:, k, c0:c0 + cn], start=(k == 0), stop=(k == 7))
                    for k in range(8):
                        R.mm(pu[:, 0:cn], wu[:, k, fc * 128:(fc + 1) * 128], HT[:, k, c0:c0 + cn], start=(k == 0), stop=(k == 7))
                    R.act(sgt[:, 0:cn], pg[:, 0:cn], AF.Silu)
                    R.tt("dve", AT[:, fc, c0:c0 + cn], sgt[:, 0:cn], pu[:, 0:cn], ALU.mult)
            for (t, col0, gidx) in tiles:
                for nh in range(2):
                    pb = PS[4 + nh + 2 * (t % 2)]
                    for fc in range(gs):
                        R.mm(pb[:, :], AT[:, fc, col0:col0 + 128], wd[:, fc, nh * 512:(nh + 1) * 512], start=(fc == 0), stop=(fc == gs - 1))
                    tm = _cm['tmp'][t % 2][:, nh * 512:(nh + 1) * 512]
                    xs = _cm['Xt'](t)[:, nh * 512:(nh + 1) * 512]
                    R.tt("dve", tm, pb[:, :], gbcs[gidx][:, nh * 512:(nh + 1) * 512], ALU.mult)
                    if comb is None:
                        R.tt("pool", xs, xs, tm, ALU.add)
                    else:
                        R.stt("dve", xs, tm, comb(t), xs, ALU.mult, ALU.add)
        for x in Wg + Wu + Wd + Sg + [AT]:
            A.rel(x)

    if L0:
        HT0 = A.alloc([8, 2816], BF16, "HT0")
        xt = [A.alloc([1024], F32) for _ in range(4)]
        for t in range(2):
            R.dma("sp", xt[t], ctx.ap()[t * 128:(t + 1) * 128, :])
        norm_group(xt[0:2], AB[0][0], AB[0][1], 1, HT0, 0)
        for g in range(5):
            for i in range(4):
                t = g * 4 + i
                R.dma("sp", xt[i], xh.ap()[t * 128:(t + 1) * 128, :])
            norm_group(xt, AB[0][0], AB[0][1], 0, HT0, 256 + g * 512)
        for t in xt:
            A.rel(t)
        tap("HT0", HT0, [8, 2816])
        dbg_cc("B")
        if stop_after == "B":
            R.emit()
            return nc, tap_out

        QT = A.alloc([4, 2816], BF16, "QT")
        KT = A.alloc([4, 2816], BF16, "KT")
        VA = A.alloc([22, 8 * 65], BF16, "VA")
        MIX = A.alloc([18, 1024], BF16, "MIX")
        R.memset("pool", VA, 1.0)
        blocks = [(b * 512, 512) for b in range(5)] + [(2560, 256)]
        W = A.alloc([8, 1024], BF16, "Wqk")
        R.dma("pool", W, na_w_in.ap()[:, 0:1024].rearrange("(k p) c -> p k c", p=128))
        ev = 0
        for which, DST in ((0, QT), (1, KT)):
            for j in range(4):
                for (c0, cn) in blocks:
                    pb = PS[ev % 4]
                    for k in range(8):
                        R.mm(pb[:, 0:cn], W[:, k, which * 512 + j * 128: which * 512 + (j + 1) * 128],
                             HT0[:, k, c0:c0 + cn], start=(k == 0), stop=(k == 7))
                    if which == 0:
                        R.act(DST[:, j, c0:c0 + cn], pb[:, 0:cn], AF.Identity, scale=0.125)
                    else:
                        R.cp("dve", DST[:, j, c0:c0 + cn], pb[:, 0:cn])
                    ev += 1
        A.rel(W)
        W = A.alloc([8, 512], BF16, "Wv")
        R.dma("pool", W, na_w_in.ap()[:, 1024:1536].rearrange("(k p) c -> p k c", p=128))
        for t in range(22):
            pb = PS[ev % 4]
            ev += 1
            for k in range(8):
                R.mm(pb[:, :], HT0[:, k, t * 128:(t + 1) * 128], W[:, k, :], start=(k == 0), stop=(k == 7))
            dst = VA[:, t, :].rearrange("p (h d) -> p h d", d=65)[:, :, 0:64]
            R.cp("dve" if t % 2 == 0 else "act", dst, pb[:, :].rearrange("p (h d) -> p h d", d=64))
        A.rel(W)
        tap("QT", QT, [4, 2816]); tap("KT", KT, [4, 2816]); tap("VA", VA, [22, 520])

        W = A.alloc([8, 1024], BF16, "Wug")
        R.dma("pool", W, na_w_in.ap()[:, 1536:2560].rearrange("(k p) c -> p k c", p=128))
        ws32 = A.alloc([8, 128], F32)
        ws16 = A.alloc([8, 128], BF16)
        WST = A.alloc([8, 128], BF16)
        R.dma("sp", ws32, sg_w.ap().rearrange("g p q -> p g q"))
        R.cp("dve", ws16, ws32)
        for g in range(8):
            R.tr(PSB[4][:, g * 128:(g + 1) * 128], ws16[:, g, :], ident16)
        R.cp("dve", WST, PSB[4][:, 0:1024].rearrange("p (g q) -> p g q", q=128))
        sgn_bc = A.alloc([512], F32)
        R.dma("sp", sgn_bc, sg_norm_g.ap().partition_broadcast(128))
        bs = A.alloc([8], F32)
        R.dma("sp", bs, sg_b.ap().rearrange("g p -> p g"), allow_slow_non_contiguous=True)
        bsbc = A.alloc([8, 64], F32)
        R.cp("dve", bsbc, bs.rearrange("p (g o) -> p g o", o=1).broadcast_to([128, 8, 64]))
        A.rel(ws32); A.rel(ws16)
        u_t = [A.alloc([512], F32) for _ in range(2)]
        g_t = [A.alloc([8, 64], F32) for _ in range(2)]
        gn_t = [A.alloc([512], BF16) for _ in range(2)]
        st_t = [A.alloc([32], F32) for _ in range(2)]
        for t in range(18):
            col = t * 128 if t < 2 else 512 + (t - 2) * 128
            pu, pg, pm = PS[0 + 2 * (t % 2)], PS[1 + 2 * (t % 2)], PS[6 + (t % 2)]
            for k in range(8):
                R.mm(pu[:, :], HT0[:, k, col:col + 128], W[:, k, 0:512], start=(k == 0), stop=(k == 7))
            for k in range(8):
                R.mm(pg[:, :], HT0[:, k, col:col + 128], W[:, k, 512:1024], start=(k == 0), stop=(k == 7))
            u = u_t[t % 2]; g = g_t[t % 2]; gn = gn_t[t % 2]; st = st_t[t % 2]
            g2 = g.rearrange("p g d -> p (g d)")
            R.act(u, pu[:, :], AF.Gelu_apprx_tanh)
            R.act(g2, pg[:, :], AF.Gelu_apprx_tanh)
            mean = st[:, 0:8]; var = st[:, 8:16]; rstd = st[:, 16:24]
            R.red(mean, g, ALU.add)
            R.ts("dve", mean, mean, 1.0 / 64, None, ALU.mult)
            R.tt("dve", g, g, mean.rearrange("p (g o) -> p g o", o=1).broadcast_to([128, 8, 64]), ALU.subtract)
            sq = A.alloc([8, 64], F32)
            R.tt("pool", sq, g, g, ALU.mult)
            R.red(var, sq, ALU.add)
            A.rel(sq)
            R.act(rstd, var, AF.Sqrt, scale=1.0 / 64, bias=EPS)
            R.recip(rstd, rstd)
            R.tt("dve", g, g, rstd.rearrange("p (g o) -> p g o", o=1).broadcast_to([128, 8, 64]), ALU.mult)
            R.tt("pool", gn, g2, sgn_bc, ALU.mult)
            for gi in range(8):
                R.mm(pm[:, gi * 64:(gi + 1) * 64], WST[:, gi, :], gn[:, gi * 64:(gi + 1) * 64])
            R.tt("dve", g2, pm[:, :], bsbc.rearrange("p g d -> p (g d)"), ALU.add)
            R.tt("dve", MIX[:, t, 512:1024], g2, u, ALU.mult)
        for tl in (u_t, g_t, gn_t, st_t):
            for x in tl:
                A.rel(x)
        A.rel(W); A.rel(WST); A.rel(sgn_bc); A.rel(bs); A.rel(bsbc)
        A.rel(HT0)
        tap("MIXb", MIX, [18, 1024])
        dbg_cc("D")
        if stop_after == "D":
            R.emit()
            return nc, tap_out

        def pair_cfg(p):
            if p == 0:
                return list(range(0, 6)), 0
            if p == 1:
                return list(range(1, 6)), 6
            if p == 14:
                return list(range(14, 19)), 16
            if p == 15:
                return list(range(14, 20)), 21
            return list(range(p, p + 5)), 11

        tabs = [A.alloc([27, 128], F32, "tab") for _ in range(2)]
        Ts = [A.alloc([6, 128], F32) for _ in range(3)]
        Ps = [A.alloc([8, 128], BF16) for _ in range(3)]
        rd = A.alloc([16], F32)
        it = 0
        for h in range(8):
            tab = tabs[h % 2]
            R.dma("sp", tab, nabias.ap()[h])
            j, po = h // 2, (h % 2) * 64
            for p in range(-2, 16):
                if p < 0:
                    chunks, slot0 = [], 0
                    qcol = (p + 2) * 128
                    mixt = p + 2
                else:
                    chunks, slot0 = pair_cfg(p)
                    qcol = 512 + 128 * p
                    mixt = 2 + p
                nw = len(chunks)
                T = Ts[it % 3]; Pp = Ps[it % 3]
                psa, psb = PS[(it % 3) * 2], PS[(it % 3) * 2 + 1]
                po_ps = PS[6 + (it % 2)]
                it += 1
                q = QT[po:po + 64, j, qcol:qcol + 128]
                keycols = [256 + 128 * m for m in chunks] + [0, 128]
                for s, kc in enumerate(keycols):
                    pb = psa if s < 4 else psb
                    R.mm(pb[:, (s % 4) * 128:(s % 4 + 1) * 128], KT[po:po + 64, j, kc:kc + 128], q)
                if nw:
                    n1 = min(nw, 4)
                    R.tt("dve", T[:, 0:n1, :], psa[:, 0:n1 * 128].rearrange("p (s q) -> p s q", q=128),
                         tab[:, slot0:slot0 + n1, :], ALU.add)
                    if nw > 4:
                        R.tt("dve", T[:, 4:nw, :], psb[:, 0:(nw - 4) * 128].rearrange("p (s q) -> p s q", q=128),
                             tab[:, slot0 + 4:slot0 + nw, :], ALU.add)
                    R.act(Pp[:, 0:nw, :], T[:, 0:nw, :], AF.Exp)
                for s in range(nw, nw + 2):
                    pb = psa if s < 4 else psb
                    R.act(Pp[:, s, :], pb[:, (s % 4) * 128:(s % 4 + 1) * 128], AF.Exp)
                vt = [2 + m for m in chunks] + [0, 1]
                for s, t in enumerate(vt):
                    R.mm(po_ps[:, 0:65], Pp[:, s, :], VA[:, t, h * 65:(h + 1) * 65], start=(s == 0), stop=(s == nw + 1))
                rc = rd[:, (it % 16):(it % 16) + 1]
                R.recip(rc, po_ps[:, 64:65])
                R.ts("dve", MIX[:, mixt, h * 64:(h + 1) * 64], po_ps[:, 0:64], rc, None, ALU.mult)
        for x in tabs + Ts + Ps + [rd]:
            A.rel(x)
        A.rel(QT); A.rel(KT); A.rel(VA)
        tap("MIX", MIX, [18, 1024])
        dbg_cc("E")
        if stop_after == "E":
            R.emit()
            return nc, tap_out

        XC, XO, Xt, gb, tmp = alloc_common()
        for t in range(2):
            R.dma("sp", XC[:, t, :], ctx.ap()[t * 128:(t + 1) * 128, :])
        for t in range(16):
            R.dma("sp", XO[:, t, :], xh.ap()[256 + t * 128:256 + (t + 1) * 128, :])
        gate_bc(gb[0], 0, 2, 0)
        gate_bc(gb[1], 0, 2, 1)
        WO = A.alloc([8, 1024], BF16, "WO")
        R.dma("pool", WO, even_w_out.ap().rearrange("(k p) c -> p k c", p=128))
        MTs = [A.alloc([8, 128], BF16) for _ in range(2)]

        def proj_residual(t, MT, Wt, gbc, nk):
            for nh in range(2):
                pb = PS[nh + 2 * (t % 2)]
                for k in range(nk):
                    R.mm(pb[:, :], MT[:, k, :], Wt[:, k, nh * 512:(nh + 1) * 512], start=(k == 0), stop=(k == nk - 1))
                tm = tmp[t % 2][:, nh * 512:(nh + 1) * 512]
                R.tt("dve", tm, pb[:, :], gbc[:, nh * 512:(nh + 1) * 512], ALU.mult)
                R.tt("pool", Xt(t)[:, nh * 512:(nh + 1) * 512], Xt(t)[:, nh * 512:(nh + 1) * 512], tm, ALU.add)

        for t in range(18):
            MT = MTs[t % 2]
            pt = PSB[4 + (t % 2)]
            for k in range(8):
                R.tr(pt[:, k * 128:(k + 1) * 128], MIX[:, t, k * 128:(k + 1) * 128], ident16)
            R.cp("act", MT, pt[:, 0:1024].rearrange("p (k q) -> p k q", q=128))
            proj_residual(t, MT, WO, gb[1] if t < 2 else gb[0], 8)
        A.rel(WO); A.rel(MIX)
        for x in MTs:
            A.rel(x)
        tap("X1c", XC, [2, 1024]); tap("X1", XO, [16, 1024])
        dbg_cc("F")
        if stop_after == "F":
            R.emit()
            return nc, tap_out

        HTf = A.alloc([8, 2304], BF16, "HTf")
        norm_group([Xt(0), Xt(1)], AB[0][2], AB[0][3], 1, HTf, 0)
        for g in range(4):
            norm_group([Xt(2 + g * 4 + i) for i in range(4)], AB[0][2], AB[0][3], 0, HTf, 256 + g * 512)
        gate_bc(gb[0], 0, 5, 0)
        gate_bc(gb[1], 0, 5, 1)
        ffn(HTf, 2304, [(t, t * 128, 1 if t < 2 else 0) for t in range(18)],
            ffn_w_gate.ap(), ffn_w_up.ap(), ffn_w_down.ap(), 22, gb)
        A.rel(HTf)
        tap("X2c", XC, [2, 1024]); tap("X2", XO, [16, 1024])
        dbg_cc("G")
        if stop_after == "G":
            R.emit()
            return nc, tap_out

        if which == "l0":
            for t_ in range(2):
                R.dma("sp", c2_d.ap()[t_ * 128:(t_ + 1) * 128, :], XC[:, t_, :])
            for t_ in range(16):
                R.dma("sp", x2_d.ap()[t_ * 128:(t_ + 1) * 128, :], XO[:, t_, :])
            R.emit()
            return nc, tap_out

    if not FUSED:
        XC, XO, Xt, gb, tmp = alloc_common()
        A.rel(XC)
        for t_ in range(16):
            R.dma("sp", XO[:, t_, :], xo.ap()[t_ * 128:(t_ + 1) * 128, :])
    W1 = A.alloc([8, 672], BF16, "W1")
    W1s = A.alloc([8, 32], BF16, "W1s")
    R.dma("pool", W1, mla_w_in.ap().rearrange("(k p) c -> p k c", p=128))
    R.dma("pool", W1s, mla_w_kr_sw.ap().rearrange("(k p) c -> p k c", p=128))
    HT1 = A.alloc([8, 2048], BF16, "HT1")
    for g in range(4):
        norm_group([XO[:, g * 4 + i, :] for i in range(4)], AB[1][0], AB[1][1], 0, HT1, g * 512)
    tap("HT1", HT1, [8, 2048])
    if stop_after == "Hn":
        R.emit()
        return nc, tap_out
    sq = [A.alloc([512], BF16) for _ in range(2)]
    rst = A.alloc([512], F32)
    t1 = A.alloc([512], F32)
    t2 = A.alloc([512], F32)
    CQ = A.alloc([3, 2048], BF16, "CQ")
    CR = A.alloc([2048], F32, "CR")
    SR = A.alloc([2048], F32, "SR")
    R.dma("sp", CR[0:96, :], ropeq.ap()[0])
    R.dma("sp", SR[0:96, :], ropeq.ap()[1])
    sq3 = A.alloc([512], BF16)
    import os
    QSL = int(os.environ.get("QS", "4"))
    for b in range(4):
        c0 = 512 * b
        pc = [PS[0], PS[1], PS[2]]
        pss = PS[3]
        sqs = [sq[0], sq[1], sq3]
        for r in range(3):
            for k in range(8):
                R.mm(pc[r][:, :], W1[:, k, r * 128:(r + 1) * 128], HT1[:, k, c0:c0 + 512], start=(k == 0), stop=(k == 7))
            if QSL >= 2:
                R.act(sqs[r], pc[r][:, :], AF.Square)
            R.cp("dve", CQ[:, r, b * 512:(b + 1) * 512], pc[r][:, :])
        if QSL >= 3:
            for r in range(3):
                R.mm(pss[:, :], ones16, sqs[r], start=(r == 0), stop=(r == 2))
            R.act(rst, pss[:, :], AF.Sqrt, scale=1.0 / 384, bias=EPS)
            R.recip(rst, rst)
        if QSL >= 4:
            R.tt("dve", CR[0:96, b * 512:(b + 1) * 512], CR[0:96, b * 512:(b + 1) * 512], rst[0:96, :], ALU.mult)
            R.tt("pool", SR[0:96, b * 512:(b + 1) * 512], SR[0:96, b * 512:(b + 1) * 512], rst[0:96, :], ALU.mult)
    A.rel(sq3)
    A.rel(HT1)
    tap("CQ", CQ, [3, 2048])
    if stop_after == "Hq":
        R.emit()
        return nc, tap_out

    QD = R.dram("QD", [16, 96, 2048], BF16)
    KD = R.dram("KD", [16, 64, 8448], BF16)
    VD = R.dram("VD", [16, 128, 66 * 64], BF16)
    qg = A.alloc([3], F32)
    kvg = A.alloc([2], F32)
    R.dma("sp", qg, mla_q_norm_g.ap().rearrange("(r p) -> p r", p=128), allow_slow_non_contiguous=True)
    R.dma("sp", kvg, mla_kv_norm_g.ap().rearrange("(r p) -> p r", p=128), allow_slow_non_contiguous=True)
    WQ = A.alloc([3, 1536], BF16, "WQ")
    WQS = A.alloc([3, 1536], BF16, "WQS")
    R.dma("pool", WQ, mla_w_uq.ap().rearrange("(r p) c -> p r c", p=128))
    R.dma("pool", WQS, mla_w_uq_sw.ap().rearrange("(r p) c -> p r c", p=128))
    for r in range(3):
        R.ts("dve", WQ[:, r, :], WQ[:, r, :], qg[:, r:r + 1], None, ALU.mult)
        R.ts("pool", WQS[:, r, :], WQS[:, r, :], qg[:, r:r + 1], None, ALU.mult)
    QS = [A.alloc([2048], BF16) for _ in range(2)]
    ev = 0
    for h in range(16):
        qs = QS[h % 2]
        for b in range(4):
            pq, pqs = PS[(ev % 2) * 2], PS[(ev % 2) * 2 + 1]
            ev += 1
            for r in range(3):
                R.mm(pq[0:96, :], WQ[:, r, h * 96:(h + 1) * 96], CQ[:, r, b * 512:(b + 1) * 512], start=(r == 0), stop=(r == 2))
            for r in range(3):
                R.mm(pqs[0:96, :], WQS[:, r, h * 96:(h + 1) * 96], CQ[:, r, b * 512:(b + 1) * 512], start=(r == 0), stop=(r == 2))
            R.tt("dve", t1[0:96, :], pq[0:96, :], CR[0:96, b * 512:(b + 1) * 512], ALU.mult)
            R.tt("dve", t2[0:96, :], pqs[0:96, :], SR[0:96, b * 512:(b + 1) * 512], ALU.mult)
            R.tt("pool", qs[0:96, b * 512:(b + 1) * 512], t1[0:96, :], t2[0:96, :], ALU.add)
        R.dma("sp", QD.ap()[h], qs[0:96, :])
    for x in QS + [WQ, WQS, CQ, CR, SR, qg, t1, t2, sq[0], sq[1], rst]:
        A.rel(x)

    if stop_after == "HQ":
        R.emit()
        return nc, tap_out
    CKA = A.alloc([2, 8448], BF16, "CKA")
    KRA = A.alloc([8448], BF16, "KRA")
    HTq = A.alloc([8, 512], BF16, "HTq")
    CKt = A.alloc([512], F32, "CK")
    SKt = A.alloc([512], F32, "SK")
    sq = [A.alloc([512], BF16) for _ in range(2)]
    rst = A.alloc([512], F32)
    t1 = A.alloc([512], F32)
    t2 = A.alloc([512], F32)
    xt = [A.alloc([1024], F32) for _ in range(4)] if not FUSED else []
    for u in range(5 if FUSED else 17):
        if u == 0:
            cn, dst0 = 256, 0
            if FUSED:
                norm_group([XC[:, 0, :], XC[:, 1, :]], AB[1][0], AB[1][1], 1, HTq, 0)
                A.rel(XC)
            else:
                for i in range(2):
                    R.dma("sp", xt[i], ctx.ap()[i * 128:(i + 1) * 128, :])
                norm_group(xt[0:2], AB[1][0], AB[1][1], 1, HTq, 0)
        else:
            cn, dst0 = 512, 256 + 512 * (u - 1)
            tok0 = 512 * (u - 1)
            R.dma("sp", CKt[0:32, :], ropek.ap()[0][:, tok0:tok0 + 512])
            R.dma("sp", SKt[0:32, :], ropek.ap()[1][:, tok0:tok0 + 512])
            if FUSED:
                norm_group([XO[:, 4 * (u - 1) + i, :] for i in range(4)], AB[1][0], AB[1][1], 0, HTq, 0)
            else:
                for i in range(4):
                    R.dma("sp", xt[i], xb.ap()[tok0 + i * 128:tok0 + (i + 1) * 128, :])
                norm_group(xt, AB[1][0], AB[1][1], 0, HTq, 0)
        pc = [PS[0], PS[1]]
        pss, pk, pks = PS[2], PS[3], PS[6]
        for r in range(2):
            for k in range(8):
                R.mm(pc[r][:, 0:cn], W1[:, k, 384 + r * 128:384 + (r + 1) * 128], HTq[:, k, 0:cn],
                     start=(k == 0), stop=(k == 7))
            R.act(sq[r][:, 0:cn], pc[r][:, 0:cn], AF.Square)
        for r in range(2):
            R.mm(pss[:, 0:cn], ones16, sq[r][:, 0:cn], start=(r == 0), stop=(r == 1))
        R.act(rst[:, 0:cn], pss[:, 0:cn], AF.Sqrt, scale=1.0 / 256, bias=EPS)
        R.recip(rst[:, 0:cn], rst[:, 0:cn])
        for r in range(2):
            R.tt("dve", CKA[:, r, dst0:dst0 + cn], pc[r][:, 0:cn], rst[:, 0:cn], ALU.mult)
        for k in range(8):
            R.mm(pk[0:32, 0:cn], W1[:, k, 640:672], HTq[:, k, 0:cn], start=(k == 0), stop=(k == 7))
        if u == 0:
            R.cp("act", KRA[0:32, 0:cn], pk[0:32, 0:cn])
        else:
            for k in range(8):
                R.mm(pks[0:32, 0:cn], W1s[:, k, :], HTq[:, k, 0:cn], start=(k == 0), stop=(k == 7))
            R.tt("dve", t1[0:32, 0:cn], pk[0:32, 0:cn], CKt[0:32, 0:cn], ALU.mult)
            R.tt("dve", t2[0:32, 0:cn], pks[0:32, 0:cn], SKt[0:32, 0:cn], ALU.mult)
            R.tt("pool", KRA[0:32, dst0:dst0 + cn], t1[0:32, 0:cn], t2[0:32, 0:cn], ALU.add)
    for x in xt + sq + [rst, t1, t2, CKt, SKt, HTq, W1, W1s]:
        A.rel(x)
    if FUSED:
        kvsrc = R.dram("kvsrc", [256, 2048], BF16)
        kvdst = R.dram("kvdst", [4 * 256, 2048], BF16)
        krsrc = R.dram("krsrc", [32, 2048], BF16)
        krdst = R.dram("krdst", [4 * 32, 2048], BF16)
        R.dma("sp", kvsrc.ap().rearrange("(r p) c -> p r c", p=128), CKA[:, :, 256:2304])
        R.dma("sp", krsrc.ap(), KRA[0:32, 256:2304])
        for i_, (s_, d_) in enumerate(((kvsrc, kvdst), (krsrc, krdst))):
            R.op("pool", lambda e, s_=s_, d_=d_: e.collective_compute(
                "AllGather", ALU.bypass, replica_groups=[[0, 1, 2, 3], [4, 5, 6, 7]], ins=[s_.ap()], outs=[d_.ap()]),
                [s_.ap()], [d_.ap()], dom="cc%d" % i_, step=1)
        for rk in range(4):
            R.dma("sp", CKA[:, :, 256 + 2048 * rk:256 + 2048 * (rk + 1)],
                  kvdst.ap()[256 * rk:256 * rk + 256, :].rearrange("(r p) c -> p r c", p=128))
            R.dma("sp", KRA[0:32, 256 + 2048 * rk:256 + 2048 * (rk + 1)], krdst.ap()[32 * rk:32 * rk + 32, :])
    tap("CKA", CKA, [2, 8448]); tap("KRA", KRA, [8448])
    if stop_after == "Hkv":
        R.emit()
        return nc, tap_out
    WKV = A.alloc([2, 2048], BF16, "WKV")
    R.dma("pool", WKV, mla_w_ukv.ap().rearrange("(r p) c -> p r c", p=128))
    for r in range(2):
        R.ts("dve", WKV[:, r, :], WKV[:, r, :], kvg[:, r:r + 1], None, ALU.mult)
    KS = A.alloc([8448], BF16, "KS")
    kblocks = [(512 * b, 512) for b in range(16)] + [(8192, 256)]
    ev = 0
    for h in range(16):
        for (c0, cn) in kblocks:
            pb = PS[ev % 4]
            ev += 1
            for r in range(2):
                R.mm(pb[0:64, 0:cn], WKV[:, r, h * 128:h * 128 + 64], CKA[:, r, c0:c0 + cn], start=(r == 0), stop=(r == 1))
            if ev % 2 == 0:
                R.cp("dve", KS[0:64, c0:c0 + cn], pb[0:64, 0:cn])
            else:
                R.cp("act", KS[0:64, c0:c0 + cn], pb[0:64, 0:cn])
        R.dma("sp", KD.ap()[h], KS[0:64, :])
    A.rel(KS)
    if stop_after == "HK":
        R.emit()
        return nc, tap_out
    VS = [A.alloc([4, 66 * 64], BF16)]
    WKVv = WKV.rearrange("p r (h t d) -> p r h t d", t=2, d=64)
    for hg in range(4):
        vs = VS[0].rearrange("p h (c d) -> p h c d", d=64)
        for c in range(66):
            pb = PS[4 + (ev % 4)]
            ev += 1
            for r in range(2):
                R.mm(pb[:, 0:256].rearrange("p (h d) -> p h d", d=64), CKA[:, r, c * 128:(c + 1) * 128],
                     WKVv[:, r, 4 * hg:4 * hg + 4, 1, :], start=(r == 0), stop=(r == 1))
            if ev % 2 == 0:
                R.cp("dve", vs[:, :, c, :], pb[:, 0:256].rearrange("p (h d) -> p h d", d=64))
            else:
                R.cp("act", vs[:, :, c, :], pb[:, 0:256].rearrange("p (h d) -> p h d", d=64))
        for hh in range(4):
            R.dma("sp", VD.ap()[4 * hg + hh], VS[0][:, hh, :])
    for x in VS + [WKV, CKA, kvg]:
        A.rel(x)
    if stop_after == "H":
        R.emit()
        return nc, tap_out

    KH = [A.alloc([8448], BF16, "KH") for _ in range(2)]
    for kb in KH:
        R.dma("sp", kb[64:96, :], KRA[0:32, :])
    A.rel(KRA)
    VH0 = A.alloc([66, 65], BF16, "VH0")
    VH1 = A.alloc([66, 128], BF16, "VH1")
    QH = [A.alloc([2048], BF16) for _ in range(2)]
    PT = [A.alloc([1024], BF16) for _ in range(3)]
    OTP = [A.alloc([1, 2048], BF16) for _ in range(2)]
    WO1 = A.alloc([8, 1024], BF16, "WO1")
    rd_sb = A.alloc([512], F32)
    bc_sb = A.alloc([512], F32)
    rd_hi = A.alloc([512], BF16)
    rd_lo = A.alloc([512], BF16)
    VST = A.alloc([66, 64], BF16, "VST")
    R.dma("pool", WO1, mla_w_out.ap().rearrange("(k p) c -> p k c", p=128))
    gate_bc(gb[0], 1, 2, 0)
    R.memset("pool", VH0, 1.0)
    R.memset("pool", VH1, 0.0)
    R.memset("pool", VH1[:, :, 0:1], 1.0)
    it = 0
    for h in range(16):
        kh, qh = KH[h % 2], QH[h % 2]
        odd = h % 2
        R.dma("sp", kh[0:64, :], KD.ap()[h])
        R.dma("sp", qh[0:96, :], QD.ap()[h])
        if odd:
            vh = VH1
            R.dma("sp", VST, VD.ap()[h])
            R.cp("pool", VH1[:, :, 64:128], VST)
            M, prow, r0, r1 = 128, 0, 64, 128
        else:
            vh = VH0
            R.dma("sp", VST, VD.ap()[h])
            R.cp("pool", VH0[:, :, 0:64], VST)
            M, prow, r0, r1 = 65, 64, 0, 64
        otp = OTP[(h // 2) % 2]
        for b in range(4):
            po = PS[6]
            it += 1
            q = qh[0:96, b * 512:(b + 1) * 512]
            for cp_ in range(33 + 2):
                if cp_ < 33:
                    T = PSP[cp_ % 3]
                    for j_ in range(2):
                        c = 2 * cp_ + j_
                        R.mm(T[:, j_ * 512:(j_ + 1) * 512], kh[0:96, c * 128:(c + 1) * 128], q)
                    R.act(PT[cp_ % 3], T[:, :], AF.Exp)
                if cp_ >= 2:
                    pp = cp_ - 2
                    for j_ in range(2):
                        cc = 2 * pp + j_
                        R.mm(po[0:M, :], vh[:, cc, 0:M], PT[pp % 3][:, j_ * 512:(j_ + 1) * 512],
                             start=(cc == 0), stop=(cc == 65))
            R.recip(rd_sb[prow:prow + 1, :], po[prow:prow + 1, :])
            R.cp("dve", rd_hi[prow:prow + 1, :], rd_sb[prow:prow + 1, :])
            R.tt("dve", rd_sb[prow:prow + 1, :], rd_sb[prow:prow + 1, :], rd_hi[prow:prow + 1, :], ALU.subtract)
            R.cp("dve", rd_lo[prow:prow + 1, :], rd_sb[prow:prow + 1, :])
            nb_ = 128 if odd else 64
            R.mm(PS[7][0:nb_, :], ones16[prow:prow + 1, 0:nb_], rd_hi[prow:prow + 1, :], start=True, stop=False)
            R.mm(PS[7][0:nb_, :], ones16[prow:prow + 1, 0:nb_], rd_lo[prow:prow + 1, :], start=False, stop=True)
            R.cp("act", bc_sb[r0:r1, :], PS[7][r0:r1, :])
            R.tt("dve", otp[r0:r1, 0, b * 512:(b + 1) * 512], po[r0:r1, :], bc_sb[r0:r1, :], ALU.mult)
        if odd:
            j = h // 2
            for t in range(2, 18):
                for nh in range(2):
                    pb = PS[6 + nh]
                    R.mm(pb[:, :], otp[:, 0, (t - 2) * 128:(t - 1) * 128], WO1[:, j, nh * 512:(nh + 1) * 512])
                    tm = tmp[t % 2][:, nh * 512:(nh + 1) * 512]
                    xs = Xt(t)[:, nh * 512:(nh + 1) * 512]
                    R.tt("dve", tm, pb[:, :], gb[0][:, nh * 512:(nh + 1) * 512], ALU.mult)
                    R.tt("pool", xs, xs, tm, ALU.add)
    for x in KH + QH + PT + OTP + [VH0, VH1, WO1, rd_sb, bc_sb, VST, rd_hi, rd_lo]:
        A.rel(x)
    tap("X3", XO, [16, 1024])
    if stop_after == "I":
        R.emit()
        return nc, tap_out

    HT2 = A.alloc([8, 2048], BF16, "HT2")
    WR = A.alloc([8, 8], F32)
    R.dma("sp", WR, moe_w_router.ap().rearrange("(k p) e -> p k e", p=128))
    brbc = A.alloc([8], F32)
    R.dma("sp", brbc, moe_b_router.ap().partition_broadcast(128))
    COMB = A.alloc([16, 8], F32, "COMB")
    xn32 = A.alloc([1024], F32)
    hT32 = A.alloc([8, 128], F32)
    junk = A.alloc([1024], BF16)
    st = A.alloc([16], F32)
    lg = A.alloc([8], F32)
    lg2 = A.alloc([8], F32)
    eq1 = A.alloc([8], F32)
    eq2 = A.alloc([8], F32)
    Af, Bf = AB[1][2], AB[1][3]
    for t in range(16):
        x = XO[:, t, :]
        ss, rs, m1, m2, dd, ee, w1, w2 = [st[:, i:i + 1] for i in range(8)]
        R.act(junk, x, AF.Square, accum_out=ss)
        R.act(rs, ss, AF.Sqrt, scale=1.0 / 1024, bias=EPS)
        R.recip(rs, rs)
        R.ts("dve", xn32, x, rs, None, ALU.mult)
        for k in range(8):
            pb = PS[k // 4]
            R.tr(pb[:, (k % 4) * 128:(k % 4 + 1) * 128], xn32[:, k * 128:(k + 1) * 128], ident32)
        for k in range(8):
            pb = PS[k // 4]
            src = pb[:, (k % 4) * 128:(k % 4 + 1) * 128]
            if k % 2 == 0:
                R.ts("dve", hT32[:, k, :], src, Af[:, k, 0:1], Bf[:, k, 0:1], ALU.mult, ALU.add)
            else:
                R.act(hT32[:, k, :], src, AF.Identity, scale=Af[:, k, 0:1], bias=Bf[:, k, 0:1])
        R.cp("pool", HT2[:, :, t * 128:(t + 1) * 128], hT32)
        pl = PS[2]
        for k in range(8):
            R.mm(pl[:, 0:8], hT32[:, k, :], WR[:, k, :], start=(k == 0), stop=(k == 7))
        R.tt("dve", lg, pl[:, 0:8], brbc, ALU.add)
        R.red(m1, lg, ALU.max)
        R.ts("dve", eq1, lg, m1, None, ALU.is_equal)
        R.stt("dve", lg2, eq1, -1e30, lg, ALU.mult, ALU.add)
        R.red(m2, lg2, ALU.max)
        R.ts("dve", eq2, lg2, m2, None, ALU.is_equal)
        R.tt("dve", dd, m2, m1, ALU.subtract)
        R.act(ee, dd, AF.Exp)
        R.ts("dve", w1, ee, 1.0, None, ALU.add)
        R.recip(w1, w1)
        R.tt("dve", w2, ee, w1, ALU.mult)
        R.ts("dve", eq1, eq1, w1, None, ALU.mult)
        R.stt("dve", COMB[:, t, :], eq2, w2, eq1, ALU.mult, ALU.add)
    for x in (xn32, hT32, junk, lg, lg2, eq1, eq2, WR, brbc):
        A.rel(x)
    tap("COMB", COMB, [16, 8])
    gate_bc(gb[0], 1, 5, 0)
    ne = 8 if stop_after != "J1" else 1
    for e in range(ne):
        ffn(HT2, 2048, [(t, (t - 2) * 128, 0) for t in range(2, 18)],
            moe_w_gate.ap()[e], moe_w_up.ap()[e], moe_w_down.ap()[e], 28, gb,
            comb=lambda t, e=e: COMB[:, t - 2, e:e + 1])
    A.rel(HT2)

    fgbc = A.alloc([1024], F32)
    R.dma("sp", fgbc, final_g.ap().partition_broadcast(128))
    ot = [A.alloc([1024], F32) for _ in range(2)]
    junk = A.alloc([1024], BF16)
    for t in range(16):
        x = XO[:, t, :]
        ss, rs = st[:, 8:9], st[:, 9:10]
        R.act(junk, x, AF.Square, accum_out=ss)
        R.act(rs, ss, AF.Sqrt, scale=1.0 / 1024, bias=EPS)
        R.recip(rs, rs)
        R.stt("dve", ot[t % 2], x, rs, fgbc, ALU.mult, ALU.mult)
        R.dma("sp", out_d.ap()[t * 128:(t + 1) * 128, :], ot[t % 2])
    R.emit()
    return nc, tap_out


def _na_bias_tables(rpb, cpos):
    NEG = np.float32(-30000.0)
    tab = np.full((8, 27, 128, 128), NEG, np.float32)
    rows_b = 128

    def cfg(p):
        if p == 0:
            return list(range(0, 6)), 0
        if p == 1:
            return list(range(1, 6)), 6
        if p == 14:
            return list(range(14, 19)), 16
        if p == 15:
            return list(range(14, 20)), 21
        return list(range(p, p + 5)), 11

    cols = np.arange(64)
    cstart = np.clip(cols - 8, 0, 48)
    for p in (0, 1, 2, 14, 15):
        chunks, slot0 = cfg(p)
        for s, m in enumerate(chunks):
            for kr_l in range(2):
                jrow = 2 * m + kr_l
                grow = 32 * cpos - 4 + jrow
                for qr_l in range(2):
                    i = 2 * p + qr_l
                    r = 32 * cpos + i
                    rs = min(max(r - 4, 0), rows_b - 8)
                    if grow < rs or grow >= rs + 8 or grow < 0 or grow >= rows_b:
                        continue
                    ridx = grow - r + 7
                    kc = cols[:, None]
                    qc = cols[None, :]
                    valid = (kc >= cstart[None, :]) & (kc < cstart[None, :] + 16)
                    cidx = np.clip(kc - qc + 15, 0, 30)
                    vals = rpb[:, ridx, :][:, cidx]
                    blk = np.where(valid[None], vals, NEG)
                    tab[:, slot0 + s, kr_l * 64:(kr_l + 1) * 64, qr_l * 64:(qr_l + 1) * 64] = blk
    return np.ascontiguousarray(tab.transpose(0, 2, 1, 3))


def _rope_tables(cpos):
    t = np.arange(2048, dtype=np.int64) + 2048 * cpos
    pos = np.stack([t // GRID_W, t % GRID_W], -1).astype(np.float32)
    inv = np.power(np.float32(10000.0), -np.arange(8, dtype=np.float32) / np.float32(8)).astype(np.float32)
    ang = pos[:, :, None] * inv
    cos = np.cos(ang).astype(np.float32)
    sin = np.sin(ang).astype(np.float32)
    C = np.zeros((32, 2048), np.float32)
    S = np.zeros((32, 2048), np.float32)
    for a in range(2):
        for half in range(2):
            rows = slice(a * 16 + half * 8, a * 16 + half * 8 + 8)
            C[rows] = cos[:, a, :].T
            S[rows] = (-sin[:, a, :].T) if half == 0 else sin[:, a, :].T
    scale = np.float32(96.0 ** -0.5)
    Cq = np.concatenate([np.full((64, 2048), scale, np.float32), C * scale], 0)
    Sq = np.concatenate([np.zeros((64, 2048), np.float32), S * scale], 0)
    return np.stack([Cq, Sq]), np.stack([C, S])


_SWAP32 = np.concatenate([np.arange(8, 16), np.arange(0, 8), np.arange(24, 32), np.arange(16, 24)])


def _rope_k_full():
    t = np.arange(8192, dtype=np.int64)
    pos = np.stack([t // GRID_W, t % GRID_W], -1).astype(np.float32)
    inv = np.power(np.float32(10000.0), -np.arange(8, dtype=np.float32) / np.float32(8)).astype(np.float32)
    ang = pos[:, :, None] * inv
    cos = np.cos(ang).astype(np.float32)
    sin = np.sin(ang).astype(np.float32)
    C = np.zeros((32, 8192), np.float32)
    S = np.zeros((32, 8192), np.float32)
    for a in range(2):
        for half in range(2):
            rows = slice(a * 16 + half * 8, a * 16 + half * 8 + 8)
            C[rows] = cos[:, a, :].T
            S[rows] = (-sin[:, a, :].T) if half == 0 else sin[:, a, :].T
    return np.stack([C, S])


_f = lambda a: np.ascontiguousarray(np.asarray(a, dtype=np.float32))


def make_maps_l0(inp):
    f = _f
    x = f(inp["x"]); ctxa = f(inp["ctx"])
    shared = dict(
        ada_w=f(inp["ada_w"]), ada_b=f(inp["ada_b"]), norm_g=f(inp["norm_g"]),
        na_w_in=f(inp["na_w_in"][0]), sg_w=f(inp["sg_w"][0]), sg_b=f(inp["sg_b"][0]), sg_norm_g=f(inp["sg_norm_g"][0]),
        even_w_out=f(inp["even_w_out"][0]), ffn_w_gate=f(inp["ffn_w_gate"][0]), ffn_w_up=f(inp["ffn_w_up"][0]),
        ffn_w_down=f(inp["ffn_w_down"][0]), ident=np.eye(128, dtype=np.float32),
    )
    rpb = f(inp["na_rpb"][0])
    maps = []
    for c in range(8):
        b, cp = c // 4, c % 4
        xhp = np.zeros((2560, 1024), np.float32)
        lo = 2048 * cp - 256
        s0, s1 = max(lo, 0), min(lo + 2560, 8192)
        xhp[s0 - lo:s1 - lo] = x[b, s0:s1]
        m = dict(shared)
        m.update(xh=xhp, ctx=ctxa[b], cond=np.stack([f(inp["c"])[b], f(inp["c_ctx"])]),
                 nabias=_na_bias_tables(rpb, cp))
        maps.append(m)
    return maps


def make_maps_l1(inp, x2, c2, fused=False):
    f = _f
    uq = f(inp["mla_w_uq"][0])
    perm = np.arange(1536).reshape(16, 96).copy()
    perm[:, 64:] = perm[:, 64:][:, _SWAP32]
    uq_sw = np.ascontiguousarray(uq[:, perm.reshape(-1)])
    w_in1 = f(inp["mla_w_in"][0])
    kr_sw = np.ascontiguousarray(w_in1[:, 640:672][:, _SWAP32])
    shared = dict(
        ada_w=f(inp["ada_w"]), ada_b=f(inp["ada_b"]), norm_g=f(inp["norm_g"]), final_g=f(inp["final_g"]),
        mla_w_in=w_in1, mla_w_kr_sw=kr_sw,
        mla_q_norm_g=f(inp["mla_q_norm_g"][0]), mla_kv_norm_g=f(inp["mla_kv_norm_g"][0]),
        mla_w_uq=uq, mla_w_uq_sw=uq_sw, mla_w_ukv=f(inp["mla_w_ukv"][0]), mla_w_out=f(inp["mla_w_out"][0]),
        moe_w_router=f(inp["moe_w_router"][0]), moe_b_router=f(inp["moe_b_router"][0]),
        moe_w_gate=f(inp["moe_w_gate"][0]), moe_w_up=f(inp["moe_w_up"][0]), moe_w_down=f(inp["moe_w_down"][0]),
        ident=np.eye(128, dtype=np.float32), ropek=_rope_k_full(),
    )
    maps = []
    for c in range(8):
        b, cp = c // 4, c % 4
        rq, rk_ = _rope_tables(cp)
        m = dict(shared)
        if fused:
            m.update(ropek=rk_, ropeq=rq)
        else:
            m.update(xb=np.ascontiguousarray(x2[b]), xo=np.ascontiguousarray(x2[b, 2048 * cp:2048 * (cp + 1)]),
                     ctx=np.ascontiguousarray(c2[b]), cond=np.stack([f(inp["c"])[b], f(inp["c_ctx"])]), ropeq=rq)
        maps.append(m)
    return maps


def make_maps_fused(inp):
    m0 = make_maps_l0(inp)
    x = _f(inp["x"]); ctxa = _f(inp["ctx"])
    m1 = make_maps_l1(inp, x, ctxa, fused=True)
    maps = []
    for c in range(8):
        m = dict(m1[c])
        m.update(m0[c])
        maps.append(m)
    return maps


def kernel(**inputs):
    nc, _ = build("fused")
    res = run_bass_kernel_spmd(nc, make_maps_fused(inputs), core_ids=list(range(8)))
    out = np.zeros((2, 8192, 1024), np.float32)
    for c in range(8):
        b, cp = c // 4, c % 4
        out[b, 2048 * cp:2048 * (cp + 1)] = res.results[c]["out"]
    return out
```

```python
import numpy as np
import concourse.bass as bass
import concourse.mybir as mybir
from concourse.bass_utils import run_bass_kernel_spmd

F32 = mybir.dt.float32
BF16 = mybir.dt.bfloat16
AF = mybir.ActivationFunctionType
ALU = mybir.AluOpType
AX = mybir.AxisListType
DTSZ = {F32: 4, BF16: 2}
NSLOT = 24

GRID_W = 64
_NOCC = 0
_CCFIRST = None
EPS = 1e-6


class Op:
    __slots__ = ("eng", "fn", "dom", "idx", "waits", "signal", "clock", "is_dma", "cnt", "step")


class Rec:
    def __init__(self, nc):
        self.nc = nc
        self.ops = []
        self.meta = {}
        self.live = {}
        self.known = {}
        self.dom_ops = {}
        self.slot_rr = {}
        self.slot_last = {}

    def sb(self, name, shape, dt):
        t = self.nc.alloc_sbuf_tensor(name, list(shape), dt)
        self.meta[name] = ("sb", int(np.prod(shape[1:])) * DTSZ[dt])
        return t

    def ps(self, name, shape, dt=F32):
        t = self.nc.alloc_psum_tensor(name, list(shape), dt)
        self.meta[name] = ("ps", int(np.prod(shape[1:])) * DTSZ[dt])
        return t

    def dram(self, name, shape, dt, kind="Internal"):
        t = self.nc.dram_tensor(name, list(shape), dt, kind=kind)
        self.meta[name] = ("dram", 0)
        return t

    def box(self, ap):
        name = ap.tensor.name
        kind, row = self.meta[name]
        sz = DTSZ[ap.dtype]
        off = int(ap.offset) * sz
        dims = ap.ap
        if kind == "dram":
            hi = off + sum((c - 1) * abs(s) for s, c in dims) * sz + sz - 1
            return (name, 0, 0, off, hi)
        p0 = off // row
        f0 = off % row
        ps_, pc = dims[0]
        pstep = (abs(ps_) * sz) // row if pc > 1 else 0
        p1 = p0 + (pc - 1) * pstep
        f1 = f0 + sum((c - 1) * abs(s) for s, c in dims[1:]) * sz + sz - 1
        return (name, p0, p1, f0, f1)

    @staticmethod
    def _ovl(a, b):
        return a[1] <= b[2] and b[1] <= a[2] and a[3] <= b[4] and b[3] <= a[4]

    @staticmethod
    def _cov(a, b):
        return a[1] <= b[1] and a[2] >= b[2] and a[3] <= b[3] and a[4] >= b[4]

    def op(self, eng, fn, reads=(), writes=(), dma=False, dom=None, step=None):
        o = Op()
        o.eng = eng
        o.fn = fn
        o.is_dma = dma
        o.signal = False
        o.waits = {}
        E = eng
        known = self.known.setdefault(E, {})
        deps = {}
        rb = [self.box(a) for a in reads]
        wb = [self.box(a) for a in writes]
        psb = set()
        for lst_ in (rb, wb):
            for i_, b in enumerate(lst_):
                if self.meta[b[0]][0] == "ps":
                    lst_[i_] = (b[0], 0, 127, (b[3] // 2048) * 2048, (b[4] // 2048) * 2048 + 2047)
                    psb.add(b[0])
        for b in rb:
            ps_ = b[0] in psb
            for (lb, lop, lw) in self.live.get(b[0], ()):
                if self._ovl(b, lb):
                    if lw:
                        deps[lop] = True
                    elif ps_ and lop.eng != E:
                        deps[lop] = True
        for b in wb:
            for (lb, lop, lw) in self.live.get(b[0], ()):
                if self._ovl(b, lb):
                    deps[lop] = deps.get(lop, False) or lw
        if dma:
            rr = self.slot_rr.get(E, 0)
            self.slot_rr[E] = rr + 1
            dom = ("dma", E, rr % NSLOT)
            prev = self.slot_last.get(dom)
            if prev is not None:
                deps[prev] = True
            self.slot_last[dom] = o
        elif dom is None:
            dom = E
        o.dom = dom
        o.step = step if step is not None else (16 if dma else 1)
        for y, hard in deps.items():
            if y.dom == E:
                if E == "pe" or not hard:
                    continue
            if known.get(y.dom, -1) >= y.idx:
                continue
            if o.waits.get(y.dom, (-1, None))[0] < y.idx:
                o.waits[y.dom] = (y.idx, y)
        for d, (i, y) in o.waits.items():
            y.signal = True
            for k, v in y.clock.items():
                if known.get(k, -1) < v:
                    known[k] = v
        lst = self.dom_ops.setdefault(dom, [])
        o.idx = len(lst)
        lst.append(o)
        o.clock = dict(known)
        o.clock[dom] = o.idx
        for b in wb:
            l = self.live.setdefault(b[0], [])
            l[:] = [x for x in l if not self._cov(b, x[0])]
            l.append((b, o, True))
        for b in rb:
            l = self.live.setdefault(b[0], [])
            l[:] = [x for x in l if not (x[1].dom == dom and not x[2] and x[0] == b)]
            l.append((b, o, False))
        self.ops.append(o)
        return o

    def mm(self, out, lhsT, rhs, start=True, stop=True):
        return self.op("pe", lambda e: e.matmul(out, lhsT, rhs, start=start, stop=stop), [lhsT, rhs], [out])

    def tr(self, out, in_, ident):
        return self.op("pe", lambda e: e.transpose(out, in_, ident), [in_, ident], [out])

    def act(self, out, in_, func, bias=None, scale=None, accum_out=None):
        kw = {}
        rd = [in_]
        wr = [out]
        if bias is not None:
            kw["bias"] = bias
            if not isinstance(bias, (int, float)):
                rd.append(bias)
        if scale is not None:
            kw["scale"] = scale
            if not isinstance(scale, (int, float)):
                rd.append(scale)
        if accum_out is not None:
            kw["accum_out"] = accum_out
            wr.append(accum_out)
        return self.op("act", lambda e: e.activation(out, in_, func, **kw), rd, wr)

    def tt(self, eng, out, in0, in1, op):
        return self.op(eng, lambda e: e.tensor_tensor(out, in0, in1, op), [in0, in1], [out])

    def ts(self, eng, out, in0, s1, s2, op0, op1=None, accum_out=None):
        rd = [in0] + [s for s in (s1, s2) if s is not None and not isinstance(s, (int, float))]
        wr = [out] + ([accum_out] if accum_out is not None else [])
        kw = {}
        if op1 is not None:
            kw["op1"] = op1
        if accum_out is not None:
            kw["accum_out"] = accum_out
        return self.op(eng, lambda e: e.tensor_scalar(out, in0, s1, s2, op0, **kw), rd, wr)

    def stt(self, eng, out, in0, scalar, in1, op0, op1):
        rd = [in0, in1] + ([scalar] if not isinstance(scalar, (int, float)) else [])
        return self.op(eng, lambda e: e.scalar_tensor_tensor(out, in0, scalar, in1, op0, op1), rd, [out])

    def cp(self, eng, out, in_):
        if eng == "act":
            return self.op(eng, lambda e: e.copy(out, in_), [in_], [out])
        return self.op(eng, lambda e: e.tensor_copy(out, in_), [in_], [out])

    def memset(self, eng, out, val):
        return self.op(eng, lambda e: e.memset(out, val), [], [out])

    def recip(self, out, in_):
        return self.op("dve", lambda e: e.reciprocal(out, in_), [in_], [out])

    def red(self, out, in_, op):
        return self.op("dve", lambda e: e.tensor_reduce(out, in_, AX.X, op), [in_], [out])

    def dma(self, q, out, in_, **kw):
        return self.op(q, lambda e: e.dma_start(out, in_, **kw), [in_], [out], dma=True)

    def emit(self):
        nc = self.nc
        tails = {}
        for dom, last in self.slot_last.items():
            tails.setdefault(dom[1], []).append(last)
            last.signal = True
        sems = {}
        for dom, lst in self.dom_ops.items():
            nm = "s_" + ("_".join(str(x) for x in dom) if isinstance(dom, tuple) else dom)
            sems[dom] = nc.alloc_semaphore(nm)
            c = 0
            for o in lst:
                if o.signal:
                    c += o.step
                o.cnt = c
        per_eng = {}
        for o in self.ops:
            per_eng.setdefault(o.eng, []).append(o)
        engmap = {"pe": "tensor", "act": "scalar", "dve": "vector", "pool": "gpsimd", "sp": "sync"}
        with nc.Block() as block:
            for E, lst in per_eng.items():
                def body(e, lst=lst, E=E):
                    for o in lst:
                        for d, (i, y) in o.waits.items():
                            e.wait_ge(sems[d], y.cnt)
                        ins = o.fn(e)
                        if o.signal:
                            ins.then_inc(sems[o.dom], o.step)
                    for last in tails.get(E, ()):
                        e.wait_ge(sems[last.dom], last.cnt)
                getattr(block, engmap[E])(body)


class Arena:
    def __init__(self, R, cap):
        self.t = R.sb("arena", [128, cap // 2], BF16)
        self.free = [(0, cap)]
        self.used = {}

    def alloc(self, shape, dt, tag=None):
        n = int(np.prod(shape)) * DTSZ[dt]
        na = (n + 63) // 64 * 64
        for i, (o, s) in enumerate(self.free):
            if s >= na:
                self.free[i] = (o + na, s - na)
                break
        else:
            raise MemoryError(f"arena full: want {na} free {self.free} tag {tag}")
        ap = self.t[:, o // 2:(o + n) // 2]
        if dt != BF16:
            ap = ap.bitcast(dt)
        if len(shape) == 2:
            ap = ap.rearrange("p (a b) -> p a b", b=shape[1])
        elif len(shape) == 3:
            ap = ap.rearrange("p (a b c) -> p a b c", b=shape[1], c=shape[2])
        self.used[id(ap)] = (o, na)
        ap_key = ap
        self._last = (o, na)
        return ap

    def rel(self, ap):
        o, na = self.used.pop(id(ap))
        self.free.append((o, na))
        self.free.sort()
        m = []
        for o, s in self.free:
            if m and m[-1][0] + m[-1][1] == o:
                m[-1] = (m[-1][0], m[-1][1] + s)
            else:
                m.append((o, s))
        self.free = [x for x in m if x[1] > 0]


def build(which, stop_after=None, taps=()):
    nc = bass.Bass("TRN2", target_bir_lowering=False)
    R = Rec(nc)
    A = Arena(R, 206 * 1024)
    PSP = [R.ps(f"psp{i}", [128, 1024], F32) for i in range(4)]
    PS = [PSP[i // 2][:, (i % 2) * 512:(i % 2 + 1) * 512] for i in range(8)]
    PSB = [p.bitcast(BF16) for p in PS]
    tap_out = {}

    def din(name, shape):
        return R.dram(name, shape, F32, kind="ExternalInput")

    L0 = which in ("l0", "fused")
    L1 = which in ("l1", "fused")
    FUSED = which == "fused"
    d0 = lambda n, sh: din(n, sh) if L0 else None
    d1 = lambda n, sh: din(n, sh) if L1 else None
    xh = d0("xh", [2560, 1024])
    ctx = din("ctx", [256, 1024])
    cond = din("cond", [2, 1024])
    ada_w = din("ada_w", [2, 1024, 6144])
    ada_b = din("ada_b", [2, 6144])
    norm_g = din("norm_g", [2, 2, 1024])
    final_g = d1("final_g", [1024])
    na_w_in = d0("na_w_in", [1024, 2560])
    nabias = d0("nabias", [8, 128, 27, 128])
    sg_w = d0("sg_w", [8, 128, 128])
    sg_b = d0("sg_b", [8, 128])
    sg_norm_g = d0("sg_norm_g", [512])
    even_w_out = d0("even_w_out", [1024, 1024])
    ffn_w_gate = d0("ffn_w_gate", [1024, 2816])
    ffn_w_up = d0("ffn_w_up", [1024, 2816])
    ffn_w_down = d0("ffn_w_down", [2816, 1024])
    xb = din("xb", [8192, 1024]) if which == "l1" else None
    xo = din("xo", [2048, 1024]) if which == "l1" else None
    mla_w_in = d1("mla_w_in", [1024, 672])
    mla_w_kr_sw = d1("mla_w_kr_sw", [1024, 32])
    mla_q_norm_g = d1("mla_q_norm_g", [384])
    mla_kv_norm_g = d1("mla_kv_norm_g", [256])
    mla_w_uq = d1("mla_w_uq", [384, 1536])
    mla_w_uq_sw = d1("mla_w_uq_sw", [384, 1536])
    mla_w_ukv = d1("mla_w_ukv", [256, 2048])
    mla_w_out = d1("mla_w_out", [1024, 1024])
    moe_w_router = d1("moe_w_router", [1024, 8])
    moe_b_router = d1("moe_b_router", [8])
    moe_w_gate = d1("moe_w_gate", [8, 1024, 3584])
    moe_w_up = d1("moe_w_up", [8, 1024, 3584])
    moe_w_down = d1("moe_w_down", [8, 3584, 1024])
    ropeq = d1("ropeq", [2, 96, 2048])
    ropek = d1("ropek", [2, 32, 2048 if FUSED else 8192])
    ident_d = din("ident", [128, 128])
    if which == "l0":
        x2_d = R.dram("x2", [2048, 1024], F32, kind="ExternalOutput")
        c2_d = R.dram("c2", [256, 1024], F32, kind="ExternalOutput")
    else:
        out_d = R.dram("out", [2048, 1024], F32, kind="ExternalOutput")

    def tap(name, ap_sb, shape):
        if name not in taps:
            return
        t = R.dram("tap_" + name, [128] + list(shape), ap_sb.dtype, kind="ExternalOutput")
        tap_out[name] = t
        R.dma("sp", t.ap(), ap_sb)

    ident32 = A.alloc([128], F32)
    ident16 = A.alloc([128], BF16)
    ones32 = A.alloc([128], F32)
    modv = [A.alloc([48, 2], F32) for _ in range(2)]
    AB = [[A.alloc([8, 2], F32) for _ in range(4)] for _ in range(2)]
    R.dma("sp", ident32, ident_d.ap())
    R.cp("dve", ident16, ident32)
    ones16 = A.alloc([128], BF16)
    R.memset("dve", ones32, 1.0)
    R.memset("dve", ones16, 1.0)

    def dbg_cc(pos):
        if _CCFIRST != pos:
            return
        d_s = R.dram("dbg_s", [32, 2048], BF16)
        d_d = R.dram("dbg_d", [128, 2048], BF16)
        R.dma("sp", d_s.ap()[:, 0:128], ident16[0:32, :])
        R.op("pool", lambda e: e.collective_compute(
            "AllGather", ALU.bypass, replica_groups=[[0, 1, 2, 3], [4, 5, 6, 7]], ins=[d_s.ap()], outs=[d_d.ap()]),
            [d_s.ap()], [d_d.ap()], dom="ccx", step=1)
        R.dma("sp", ones32[0:32, 0:64].bitcast(BF16), d_d.ap()[32:64, 0:128])
        R.memset("dve", ones32, 1.0)

    dbg_cc("A")
    condT = A.alloc([8, 2], F32)
    for r_ in range(2):
        R.dma("sp", condT[:, :, r_], cond.ap()[r_].rearrange("(k p) -> p k", p=128), allow_slow_non_contiguous=True)
    R.act(condT, condT, AF.Silu)
    stg = [A.alloc([8, 512], F32) for _ in range(2)]
    adab = A.alloc([48], F32)
    ng = A.alloc([2, 8], F32)
    psA = PS[0]
    si = 0
    for l in ((0, 1) if FUSED else ((0,) if L0 else (1,))):
        for cb in range(12):
            wt = stg[si % 2]
            si += 1
            R.dma("sp", wt, ada_w.ap()[l, :, cb * 512:(cb + 1) * 512].rearrange("(k p) c -> p k c", p=128))
            for fc in range(4):
                j = cb * 4 + fc
                for k in range(8):
                    R.mm(psA[:, (l * 48 + j) * 2:(l * 48 + j) * 2 + 2], wt[:, k, fc * 128:(fc + 1) * 128],
                         condT[:, k, :], start=(k == 0), stop=(k == 7))
        R.dma("sp", adab, ada_b.ap()[l].rearrange("(j p) -> p j", p=128), allow_slow_non_contiguous=True)
        for w_ in range(2):
            R.dma("sp", ng[:, w_, :], norm_g.ap()[l, w_].rearrange("(k p) -> p k", p=128), allow_slow_non_contiguous=True)
        R.tt("dve", modv[l], psA[:, l * 96:(l + 1) * 96].rearrange("p (j c) -> p j c", c=2),
             adab.rearrange("p (j o) -> p j o", o=1).broadcast_to([128, 48, 2]), ALU.add)
        for w in range(2):
            sh = modv[l][:, w * 24:w * 24 + 8, :]
            sc = modv[l][:, w * 24 + 8:w * 24 + 16, :]
            Am, Bm = AB[l][2 * w], AB[l][2 * w + 1]
            R.ts("dve", Am, sc, 1.0, None, ALU.add)
            R.tt("dve", Am, Am, ng[:, w, :].rearrange("p (k o) -> p k o", o=1).broadcast_to([128, 8, 2]), ALU.mult)
            R.cp("dve", Bm, sh)
    A.rel(stg[0]); A.rel(stg[1]); A.rel(adab); A.rel(ng); A.rel(condT)
    tap("modv0", modv[0], [48, 2])
    tap("AB0", AB[0][0], [8, 2])

    def gate_bc(dst, l, which, c):
        dg = A.alloc([2, 128], F32)
        for k in range(8):
            d = dg[:, k % 2, :]
            R.ts("dve", d, ident32, modv[l][:, which * 8 + k, c:c + 1], None, ALU.mult)
            pb = PS[6 + (k // 4)]
            R.mm(pb[:, (k % 4) * 128:(k % 4 + 1) * 128], ones32, d)
        R.cp("act", dst[:, 0:512], PS[6][:, :])
        R.cp("act", dst[:, 512:1024], PS[7][:, :])
        A.rel(dg)

    def norm_group(xs, Aap, Bap, c, HT, col0, want32=None):
        n = len(xs)
        ss = A.alloc([4], F32)
        rs = A.alloc([4], F32)
        junk = A.alloc([1024], BF16)
        xn = [A.alloc([1024], BF16) for _ in range(n)]
        for i, x in enumerate(xs):
            R.act(junk, x, AF.Square, accum_out=ss[:, i:i + 1])
        R.act(rs[:, 0:n], ss[:, 0:n], AF.Sqrt, scale=1.0 / 1024, bias=EPS)
        R.recip(rs[:, 0:n], rs[:, 0:n])
        for i, x in enumerate(xs):
            R.ts("dve" if i % 2 == 0 else "pool", xn[i], x, rs[:, i:i + 1], None, ALU.mult)
        for k in range(8):
            pst = PSB[4 + (k % 2)][:, 0:n * 128]
            for i in range(n):
                R.tr(pst[:, i * 128:(i + 1) * 128], xn[i][:, k * 128:(k + 1) * 128], ident16)
            dst = HT[:, k, col0:col0 + n * 128]
            if k % 2 == 0:
                R.ts("dve", dst, pst, Aap[:, k, c:c + 1], Bap[:, k, c:c + 1], ALU.mult, ALU.add)
            else:
                R.act(dst, pst, AF.Identity, scale=Aap[:, k, c:c + 1], bias=Bap[:, k, c:c + 1])
        for t in xn:
            A.rel(t)
        A.rel(junk); A.rel(rs); A.rel(ss)

    _cm = {}

    def alloc_common():
        XC = A.alloc([2, 1024], F32, "XC")
        XO = A.alloc([16, 1024], F32, "XO")

        def Xt(t):
            return XC[:, t, :] if t < 2 else XO[:, t - 2, :]

        gb = [A.alloc([1024], F32) for _ in range(2)]
        tmp = [A.alloc([1024], F32) for _ in range(2)]
        _cm.update(XC=XC, XO=XO, Xt=Xt, gb=gb, tmp=tmp)
        return XC, XO, Xt, gb, tmp

    def ffn(HT, ncols, tiles, wg_ap, wu_ap, wd_ap, nf, gbcs, comb=None):
        GS = 4
        ngrp = (nf + GS - 1) // GS
        Wg = [A.alloc([8, GS * 128], BF16) for _ in range(2)]
        Wu = [A.alloc([8, GS * 128], BF16) for _ in range(2)]
        Wd = [A.alloc([GS, 1024], BF16) for _ in range(2)]
        AT = A.alloc([GS, ncols], BF16, "AT")
        Sg = [A.alloc([512], BF16) for _ in range(2)]
        blocks = [(b, min(512, ncols - b)) for b in range(0, ncols, 512)]
        ev = 0
        for gi in range(ngrp):
            f0 = gi * GS
            gs = min(GS, nf - f0)
            wg, wu, wd = Wg[gi % 2], Wu[gi % 2], Wd[gi % 2]
            R.dma("pool", wg[:, :, 0:gs * 128], wg_ap[:, f0 * 128:(f0 + gs) * 128].rearrange("(k p) c -> p k c", p=128))
            R.dma("pool", wu[:, :, 0:gs * 128], wu_ap[:, f0 * 128:(f0 + gs) * 128].rearrange("(k p) c -> p k c", p=128))
            R.dma("pool", wd[:, 0:gs, :], wd_ap[f0 * 128:(f0 + gs) * 128, :].rearrange("(k p) c -> p k c", p=128))
            if comb is not None:
                R.tt("pool", wd[:, 0:gs, :], wd[:, 0:gs, :],
                     gbcs[0].rearrange("p (o c) -> p o c", o=1).broadcast_to([128, gs, 1024]), ALU.mult)
            for (c0, cn) in blocks:
                for fc in range(gs):
                    pg, pu = PS[(ev % 2) * 2], PS[(ev % 2) * 2 + 1]
                    sgt = Sg[ev % 2]
                    ev += 1
                    for k in range(8):
                        R.mm(pg[:, 0:cn], wg[:, k, fc * 128:(fc + 1) * 128], HT[:, k, c0:c0 + cn], start=(k == 0), stop=(k == 7))
                    for k in range(8):
                        R.mm(pu[:, 0:cn], wu[:, k, fc * 128:(fc + 1) * 128], HT[:, k, c0:c0 + cn], start=(k == 0), stop=(k == 7))
                    R.act(sgt[:, 0:cn], pg[:, 0:cn], AF.Silu)
                    R.tt("dve", AT[:, fc, c0:c0 + cn], sgt[:, 0:cn], pu[:, 0:cn], ALU.mult)
            for (t, col0, gidx) in tiles:
                for nh in range(2):
                    pb = PS[4 + nh + 2 * (t % 2)]
                    for fc in range(gs):
                        R.mm(pb[:, :], AT[:, fc, col0:col0 + 128], wd[:, fc, nh * 512:(nh + 1) * 512], start=(fc == 0), stop=(fc == gs - 1))
                    tm = _cm['tmp'][t % 2][:, nh * 512:(nh + 1) * 512]
                    xs = _cm['Xt'](t)[:, nh * 512:(nh + 1) * 512]
                    if comb is None:
                        R.tt("dve", tm, pb[:, :], gbcs[gidx][:, nh * 512:(nh + 1) * 512], ALU.mult)
                        R.tt("pool", xs, xs, tm, ALU.add)
                    else:
                        R.stt("dve", xs, pb[:, :], comb(t), xs, ALU.mult, ALU.add)
        for x in Wg + Wu + Wd + Sg + [AT]:
            A.rel(x)

    if L0:
        HT0 = A.alloc([8, 2816], BF16, "HT0")
        xt = [A.alloc([1024], F32) for _ in range(4)]
        for t in range(2):
            R.dma("sp", xt[t], ctx.ap()[t * 128:(t + 1) * 128, :])
        norm_group(xt[0:2], AB[0][0], AB[0][1], 1, HT0, 0)
        for g in range(5):
            for i in range(4):
                t = g * 4 + i
                R.dma("sp", xt[i], xh.ap()[t * 128:(t + 1) * 128, :])
            norm_group(xt, AB[0][0], AB[0][1], 0, HT0, 256 + g * 512)
        for t in xt:
            A.rel(t)
        tap("HT0", HT0, [8, 2816])
        dbg_cc("B")
        if stop_after == "B":
            R.emit()
            return nc, tap_out

        QT = A.alloc([4, 2816], BF16, "QT")
        KT = A.alloc([4, 2816], BF16, "KT")
        VA = A.alloc([22, 8 * 65], BF16, "VA")
        MIX = A.alloc([18, 1024], BF16, "MIX")
        R.memset("pool", VA, 1.0)
        blocks = [(b * 512, 512) for b in range(5)] + [(2560, 256)]
        W = A.alloc([8, 1024], BF16, "Wqk")
        R.dma("pool", W, na_w_in.ap()[:, 0:1024].rearrange("(k p) c -> p k c", p=128))
        ev = 0
        for which, DST in ((0, QT), (1, KT)):
            for j in range(4):
                for (c0, cn) in blocks:
                    pb = PS[ev % 4]
                    for k in range(8):
                        R.mm(pb[:, 0:cn], W[:, k, which * 512 + j * 128: which * 512 + (j + 1) * 128],
                             HT0[:, k, c0:c0 + cn], start=(k == 0), stop=(k == 7))
                    if which == 0:
                        R.act(DST[:, j, c0:c0 + cn], pb[:, 0:cn], AF.Identity, scale=0.125)
                    else:
                        R.cp("dve", DST[:, j, c0:c0 + cn], pb[:, 0:cn])
                    ev += 1
        A.rel(W)
        W = A.alloc([8, 512], BF16, "Wv")
        R.dma("pool", W, na_w_in.ap()[:, 1024:1536].rearrange("(k p) c -> p k c", p=128))
        for t in range(22):
            pb = PS[ev % 4]
            ev += 1
            for k in range(8):
                R.mm(pb[:, :], HT0[:, k, t * 128:(t + 1) * 128], W[:, k, :], start=(k == 0), stop=(k == 7))
            dst = VA[:, t, :].rearrange("p (h d) -> p h d", d=65)[:, :, 0:64]
            R.cp("dve" if t % 2 == 0 else "act", dst, pb[:, :].rearrange("p (h d) -> p h d", d=64))
        A.rel(W)
        tap("QT", QT, [4, 2816]); tap("KT", KT, [4, 2816]); tap("VA", VA, [22, 520])

        W = A.alloc([8, 1024], BF16, "Wug")
        R.dma("pool", W, na_w_in.ap()[:, 1536:2560].rearrange("(k p) c -> p k c", p=128))
        ws32 = A.alloc([8, 128], F32)
        ws16 = A.alloc([8, 128], BF16)
        WST = A.alloc([8, 128], BF16)
        R.dma("sp", ws32, sg_w.ap().rearrange("g p q -> p g q"))
        R.cp("dve", ws16, ws32)
        for g in range(8):
            R.tr(PSB[4][:, g * 128:(g + 1) * 128], ws16[:, g, :], ident16)
        R.cp("dve", WST, PSB[4][:, 0:1024].rearrange("p (g q) -> p g q", q=128))
        sgn_bc = A.alloc([512], F32)
        R.dma("sp", sgn_bc, sg_norm_g.ap().partition_broadcast(128))
        bs = A.alloc([8], F32)
        R.dma("sp", bs, sg_b.ap().rearrange("g p -> p g"), allow_slow_non_contiguous=True)
        bsbc = A.alloc([8, 64], F32)
        R.cp("dve", bsbc, bs.rearrange("p (g o) -> p g o", o=1).broadcast_to([128, 8, 64]))
        A.rel(ws32); A.rel(ws16)
        u_t = [A.alloc([512], F32) for _ in range(2)]
        g_t = [A.alloc([8, 64], F32) for _ in range(2)]
        gn_t = [A.alloc([512], BF16) for _ in range(2)]
        st_t = [A.alloc([32], F32) for _ in range(2)]
        for t in range(18):
            col = t * 128 if t < 2 else 512 + (t - 2) * 128
            pu, pg, pm = PS[0 + 2 * (t % 2)], PS[1 + 2 * (t % 2)], PS[6 + (t % 2)]
            for k in range(8):
                R.mm(pu[:, :], HT0[:, k, col:col + 128], W[:, k, 0:512], start=(k == 0), stop=(k == 7))
            for k in range(8):
                R.mm(pg[:, :], HT0[:, k, col:col + 128], W[:, k, 512:1024], start=(k == 0), stop=(k == 7))
            u = u_t[t % 2]; g = g_t[t % 2]; gn = gn_t[t % 2]; st = st_t[t % 2]
            g2 = g.rearrange("p g d -> p (g d)")
            R.act(u, pu[:, :], AF.Gelu_apprx_tanh)
            R.act(g2, pg[:, :], AF.Gelu_apprx_tanh)
            mean = st[:, 0:8]; var = st[:, 8:16]; rstd = st[:, 16:24]
            R.red(mean, g, ALU.add)
            R.ts("dve", mean, mean, 1.0 / 64, None, ALU.mult)
            R.tt("dve", g, g, mean.rearrange("p (g o) -> p g o", o=1).broadcast_to([128, 8, 64]), ALU.subtract)
            sq = A.alloc([8, 64], F32)
            R.tt("pool", sq, g, g, ALU.mult)
            R.red(var, sq, ALU.add)
            A.rel(sq)
            R.act(rstd, var, AF.Sqrt, scale=1.0 / 64, bias=EPS)
            R.recip(rstd, rstd)
            R.tt("dve", g, g, rstd.rearrange("p (g o) -> p g o", o=1).broadcast_to([128, 8, 64]), ALU.mult)
            R.tt("pool", gn, g2, sgn_bc, ALU.mult)
            for gi in range(8):
                R.mm(pm[:, gi * 64:(gi + 1) * 64], WST[:, gi, :], gn[:, gi * 64:(gi + 1) * 64])
            R.tt("dve", g2, pm[:, :], bsbc.rearrange("p g d -> p (g d)"), ALU.add)
            R.tt("dve", MIX[:, t, 512:1024], g2, u, ALU.mult)
        for tl in (u_t, g_t, gn_t, st_t):
            for x in tl:
                A.rel(x)
        A.rel(W); A.rel(WST); A.rel(sgn_bc); A.rel(bs); A.rel(bsbc)
        A.rel(HT0)
        tap("MIXb", MIX, [18, 1024])
        dbg_cc("D")
        if stop_after == "D":
            R.emit()
            return nc, tap_out

        def pair_cfg(p):
            if p == 0:
                return list(range(0, 6)), 0
            if p == 1:
                return list(range(1, 6)), 6
            if p == 14:
                return list(range(14, 19)), 16
            if p == 15:
                return list(range(14, 20)), 21
            return list(range(p, p + 5)), 11

        tabs = [A.alloc([27, 128], F32, "tab") for _ in range(2)]
        Ts = [A.alloc([6, 128], F32) for _ in range(2)]
        Ps = [A.alloc([8, 128], BF16) for _ in range(2)]
        rd = A.alloc([16], F32)
        it = 0
        for h in range(8):
            tab = tabs[h % 2]
            R.dma("sp", tab, nabias.ap()[h])
            j, po = h // 2, (h % 2) * 64
            for p in range(-2, 16):
                if p < 0:
                    chunks, slot0 = [], 0
                    qcol = (p + 2) * 128
                    mixt = p + 2
                else:
                    chunks, slot0 = pair_cfg(p)
                    qcol = 512 + 128 * p
                    mixt = 2 + p
                nw = len(chunks)
                T = Ts[it % 2]; Pp = Ps[it % 2]
                psa, psb = PS[(it % 2) * 2], PS[(it % 2) * 2 + 1]
                po_ps = PS[4 + (it % 2)]
                it += 1
                q = QT[po:po + 64, j, qcol:qcol + 128]
                keycols = [256 + 128 * m for m in chunks] + [0, 128]
                for s, kc in enumerate(keycols):
                    pb = psa if s < 4 else psb
                    R.mm(pb[:, (s % 4) * 128:(s % 4 + 1) * 128], KT[po:po + 64, j, kc:kc + 128], q)
                if nw:
                    n1 = min(nw, 4)
                    R.tt("dve", T[:, 0:n1, :], psa[:, 0:n1 * 128].rearrange("p (s q) -> p s q", q=128),
                         tab[:, slot0:slot0 + n1, :], ALU.add)
                    if nw > 4:
                        R.tt("dve", T[:, 4:nw, :], psb[:, 0:(nw - 4) * 128].rearrange("p (s q) -> p s q", q=128),
                             tab[:, slot0 + 4:slot0 + nw, :], ALU.add)
                    R.act(Pp[:, 0:nw, :], T[:, 0:nw, :], AF.Exp)
                for s in range(nw, nw + 2):
                    pb = psa if s < 4 else psb
                    R.act(Pp[:, s, :], pb[:, (s % 4) * 128:(s % 4 + 1) * 128], AF.Exp)
                vt = [2 + m for m in chunks] + [0, 1]
                for s, t in enumerate(vt):
                    R.mm(po_ps[:, 0:65], Pp[:, s, :], VA[:, t, h * 65:(h + 1) * 65], start=(s == 0), stop=(s == nw + 1))
                rc = rd[:, (it % 16):(it % 16) + 1]
                R.recip(rc, po_ps[:, 64:65])
                R.ts("dve", MIX[:, mixt, h * 64:(h + 1) * 64], po_ps[:, 0:64], rc, None, ALU.mult)
        for x in tabs + Ts + Ps + [rd]:
            A.rel(x)
        A.rel(QT); A.rel(KT); A.rel(VA)
        tap("MIX", MIX, [18, 1024])
        dbg_cc("E")
        if stop_after == "E":
            R.emit()
            return nc, tap_out

        XC, XO, Xt, gb, tmp = alloc_common()
        for t in range(2):
            R.dma("sp", XC[:, t, :], ctx.ap()[t * 128:(t + 1) * 128, :])
        for t in range(16):
            R.dma("sp", XO[:, t, :], xh.ap()[256 + t * 128:256 + (t + 1) * 128, :])
        gate_bc(gb[0], 0, 2, 0)
        gate_bc(gb[1], 0, 2, 1)
        WO = A.alloc([8, 1024], BF16, "WO")
        R.dma("pool", WO, even_w_out.ap().rearrange("(k p) c -> p k c", p=128))
        MTs = [A.alloc([8, 128], BF16) for _ in range(2)]

        def proj_residual(t, MT, Wt, gbc, nk):
            for nh in range(2):
                pb = PS[nh + 2 * (t % 2)]
                for k in range(nk):
                    R.mm(pb[:, :], MT[:, k, :], Wt[:, k, nh * 512:(nh + 1) * 512], start=(k == 0), stop=(k == nk - 1))
                tm = tmp[t % 2][:, nh * 512:(nh + 1) * 512]
                R.tt("dve", tm, pb[:, :], gbc[:, nh * 512:(nh + 1) * 512], ALU.mult)
                R.tt("pool", Xt(t)[:, nh * 512:(nh + 1) * 512], Xt(t)[:, nh * 512:(nh + 1) * 512], tm, ALU.add)

        for t in range(18):
            MT = MTs[t % 2]
            pt = PSB[4 + (t % 2)]
            for k in range(8):
                R.tr(pt[:, k * 128:(k + 1) * 128], MIX[:, t, k * 128:(k + 1) * 128], ident16)
            R.cp("act", MT, pt[:, 0:1024].rearrange("p (k q) -> p k q", q=128))
            proj_residual(t, MT, WO, gb[1] if t < 2 else gb[0], 8)
        A.rel(WO); A.rel(MIX)
        for x in MTs:
            A.rel(x)
        tap("X1c", XC, [2, 1024]); tap("X1", XO, [16, 1024])
        dbg_cc("F")
        if stop_after == "F":
            R.emit()
            return nc, tap_out

        HTf = A.alloc([8, 2304], BF16, "HTf")
        norm_group([Xt(0), Xt(1)], AB[0][2], AB[0][3], 1, HTf, 0)
        for g in range(4):
            norm_group([Xt(2 + g * 4 + i) for i in range(4)], AB[0][2], AB[0][3], 0, HTf, 256 + g * 512)
        gate_bc(gb[0], 0, 5, 0)
        gate_bc(gb[1], 0, 5, 1)
        ffn(HTf, 2304, [(t, t * 128, 1 if t < 2 else 0) for t in range(18)],
            ffn_w_gate.ap(), ffn_w_up.ap(), ffn_w_down.ap(), 22, gb)
        A.rel(HTf)
        tap("X2c", XC, [2, 1024]); tap("X2", XO, [16, 1024])
        dbg_cc("G")
        if stop_after == "G":
            R.emit()
            return nc, tap_out

        if which == "l0":
            for t_ in range(2):
                R.dma("sp", c2_d.ap()[t_ * 128:(t_ + 1) * 128, :], XC[:, t_, :])
            for t_ in range(16):
                R.dma("sp", x2_d.ap()[t_ * 128:(t_ + 1) * 128, :], XO[:, t_, :])
            R.emit()
            return nc, tap_out

    if not FUSED:
        XC, XO, Xt, gb, tmp = alloc_common()
        A.rel(XC)
        for t_ in range(16):
            R.dma("sp", XO[:, t_, :], xo.ap()[t_ * 128:(t_ + 1) * 128, :])
    W1 = A.alloc([8, 672], BF16, "W1")
    W1s = A.alloc([8, 32], BF16, "W1s")
    R.dma("pool", W1, mla_w_in.ap().rearrange("(k p) c -> p k c", p=128))
    R.dma("pool", W1s, mla_w_kr_sw.ap().rearrange("(k p) c -> p k c", p=128))
    HT1 = A.alloc([8, 2048], BF16, "HT1")
    for g in range(4):
        norm_group([XO[:, g * 4 + i, :] for i in range(4)], AB[1][0], AB[1][1], 0, HT1, g * 512)
    tap("HT1", HT1, [8, 2048])
    if stop_after == "Hn":
        R.emit()
        return nc, tap_out
    sq = [A.alloc([512], BF16) for _ in range(2)]
    rst = A.alloc([512], F32)
    t1 = A.alloc([512], F32)
    t2 = A.alloc([512], F32)
    CQ = A.alloc([3, 2048], BF16, "CQ")
    CR = A.alloc([2048], F32, "CR")
    SR = A.alloc([2048], F32, "SR")
    R.dma("sp", CR[0:96, :], ropeq.ap()[0])
    R.dma("sp", SR[0:96, :], ropeq.ap()[1])
    sq3 = A.alloc([512], BF16)
    import os
    QSL = int(os.environ.get("QS", "4"))
    for b in range(4):
        c0 = 512 * b
        pc = [PS[0], PS[1], PS[2]]
        pss = PS[3]
        sqs = [sq[0], sq[1], sq3]
        for r in range(3):
            for k in range(8):
                R.mm(pc[r][:, :], W1[:, k, r * 128:(r + 1) * 128], HT1[:, k, c0:c0 + 512], start=(k == 0), stop=(k == 7))
            if QSL >= 2:
                R.act(sqs[r], pc[r][:, :], AF.Square)
            R.cp("dve", CQ[:, r, b * 512:(b + 1) * 512], pc[r][:, :])
        if QSL >= 3:
            for r in range(3):
                R.mm(pss[:, :], ones16, sqs[r], start=(r == 0), stop=(r == 2))
            R.act(rst, pss[:, :], AF.Sqrt, scale=1.0 / 384, bias=EPS)
            R.recip(rst, rst)
        if QSL >= 4:
            R.tt("dve", CR[0:96, b * 512:(b + 1) * 512], CR[0:96, b * 512:(b + 1) * 512], rst[0:96, :], ALU.mult)
            R.tt("pool", SR[0:96, b * 512:(b + 1) * 512], SR[0:96, b * 512:(b + 1) * 512], rst[0:96, :], ALU.mult)
    A.rel(sq3)
    A.rel(HT1)
    tap("CQ", CQ, [3, 2048])
    if stop_after == "Hq":
        R.emit()
        return nc, tap_out

    QD = R.dram("QD", [16, 96, 2048], BF16)
    KD = R.dram("KD", [16, 64, 8448], BF16)
    VD = R.dram("VD", [16, 128, 66 * 64], BF16)
    qg = A.alloc([3], F32)
    kvg = A.alloc([2], F32)
    R.dma("sp", qg, mla_q_norm_g.ap().rearrange("(r p) -> p r", p=128), allow_slow_non_contiguous=True)
    R.dma("sp", kvg, mla_kv_norm_g.ap().rearrange("(r p) -> p r", p=128), allow_slow_non_contiguous=True)
    WQ = A.alloc([3, 1536], BF16, "WQ")
    WQS = A.alloc([3, 1536], BF16, "WQS")
    R.dma("pool", WQ, mla_w_uq.ap().rearrange("(r p) c -> p r c", p=128))
    R.dma("pool", WQS, mla_w_uq_sw.ap().rearrange("(r p) c -> p r c", p=128))
    for r in range(3):
        R.ts("dve", WQ[:, r, :], WQ[:, r, :], qg[:, r:r + 1], None, ALU.mult)
        R.ts("pool", WQS[:, r, :], WQS[:, r, :], qg[:, r:r + 1], None, ALU.mult)
    QS = [A.alloc([2048], BF16) for _ in range(2)]
    ev = 0
    for h in range(16):
        qs = QS[h % 2]
        for b in range(4):
            pq, pqs = PS[(ev % 2) * 2], PS[(ev % 2) * 2 + 1]
            ev += 1
            for r in range(3):
                R.mm(pq[0:96, :], WQ[:, r, h * 96:(h + 1) * 96], CQ[:, r, b * 512:(b + 1) * 512], start=(r == 0), stop=(r == 2))
            for r in range(3):
                R.mm(pqs[0:96, :], WQS[:, r, h * 96:(h + 1) * 96], CQ[:, r, b * 512:(b + 1) * 512], start=(r == 0), stop=(r == 2))
            R.tt("dve", t1[0:96, :], pq[0:96, :], CR[0:96, b * 512:(b + 1) * 512], ALU.mult)
            R.tt("dve", t2[0:96, :], pqs[0:96, :], SR[0:96, b * 512:(b + 1) * 512], ALU.mult)
            R.tt("pool", qs[0:96, b * 512:(b + 1) * 512], t1[0:96, :], t2[0:96, :], ALU.add)
        R.dma("sp", QD.ap()[h], qs[0:96, :])
    for x in QS + [WQ, WQS, CQ, CR, SR, qg, t1, t2, sq[0], sq[1], rst]:
        A.rel(x)

    if stop_after == "HQ":
        R.emit()
        return nc, tap_out
    CKA = A.alloc([2, 8448], BF16, "CKA")
    KRA = A.alloc([8448], BF16, "KRA")
    HTq = A.alloc([8, 512], BF16, "HTq")
    CKt = A.alloc([512], F32, "CK")
    SKt = A.alloc([512], F32, "SK")
    sq = [A.alloc([512], BF16) for _ in range(2)]
    rst = A.alloc([512], F32)
    t1 = A.alloc([512], F32)
    t2 = A.alloc([512], F32)
    xt = [A.alloc([1024], F32) for _ in range(4)] if not FUSED else []
    for u in range(5 if FUSED else 17):
        if u == 0:
            cn, dst0 = 256, 0
            if FUSED:
                norm_group([XC[:, 0, :], XC[:, 1, :]], AB[1][0], AB[1][1], 1, HTq, 0)
                A.rel(XC)
            else:
                for i in range(2):
                    R.dma("sp", xt[i], ctx.ap()[i * 128:(i + 1) * 128, :])
                norm_group(xt[0:2], AB[1][0], AB[1][1], 1, HTq, 0)
        else:
            cn, dst0 = 512, 256 + 512 * (u - 1)
            tok0 = 512 * (u - 1)
            R.dma("sp", CKt[0:32, :], ropek.ap()[0][:, tok0:tok0 + 512])
            R.dma("sp", SKt[0:32, :], ropek.ap()[1][:, tok0:tok0 + 512])
            if FUSED:
                norm_group([XO[:, 4 * (u - 1) + i, :] for i in range(4)], AB[1][0], AB[1][1], 0, HTq, 0)
            else:
                for i in range(4):
                    R.dma("sp", xt[i], xb.ap()[tok0 + i * 128:tok0 + (i + 1) * 128, :])
                norm_group(xt, AB[1][0], AB[1][1], 0, HTq, 0)
        pc = [PS[0], PS[1]]
        pss, pk, pks = PS[2], PS[3], PS[6]
        for r in range(2):
            for k in range(8):
                R.mm(pc[r][:, 0:cn], W1[:, k, 384 + r * 128:384 + (r + 1) * 128], HTq[:, k, 0:cn],
                     start=(k == 0), stop=(k == 7))
            R.act(sq[r][:, 0:cn], pc[r][:, 0:cn], AF.Square)
        for r in range(2):
            R.mm(pss[:, 0:cn], ones16, sq[r][:, 0:cn], start=(r == 0), stop=(r == 1))
        R.act(rst[:, 0:cn], pss[:, 0:cn], AF.Sqrt, scale=1.0 / 256, bias=EPS)
        R.recip(rst[:, 0:cn], rst[:, 0:cn])
        for r in range(2):
            R.tt("dve", CKA[:, r, dst0:dst0 + cn], pc[r][:, 0:cn], rst[:, 0:cn], ALU.mult)
        for k in range(8):
            R.mm(pk[0:32, 0:cn], W1[:, k, 640:672], HTq[:, k, 0:cn], start=(k == 0), stop=(k == 7))
        if u == 0:
            R.cp("act", KRA[0:32, 0:cn], pk[0:32, 0:cn])
        else:
            for k in range(8):
                R.mm(pks[0:32, 0:cn], W1s[:, k, :], HTq[:, k, 0:cn], start=(k == 0), stop=(k == 7))
            R.tt("dve", t1[0:32, 0:cn], pk[0:32, 0:cn], CKt[0:32, 0:cn], ALU.mult)
            R.tt("dve", t2[0:32, 0:cn], pks[0:32, 0:cn], SKt[0:32, 0:cn], ALU.mult)
            R.tt("pool", KRA[0:32, dst0:dst0 + cn], t1[0:32, 0:cn], t2[0:32, 0:cn], ALU.add)
    for x in xt + sq + [rst, t1, t2, CKt, SKt, HTq, W1, W1s]:
        A.rel(x)
    if FUSED:
        kvsrc = R.dram("kvsrc", [256, 2048], BF16)
        kvdst = R.dram("kvdst", [4 * 256, 2048], BF16)
        krsrc = R.dram("krsrc", [32, 2048], BF16)
        krdst = R.dram("krdst", [4 * 32, 2048], BF16)
        R.dma("sp", kvsrc.ap().rearrange("(r p) c -> p r c", p=128), CKA[:, :, 256:2304])
        R.dma("sp", krsrc.ap(), KRA[0:32, 256:2304])
        for i_, (s_, d_) in enumerate(((kvsrc, kvdst), (krsrc, krdst))):
            R.op("pool", lambda e, s_=s_, d_=d_: e.collective_compute(
                "AllGather", ALU.bypass, replica_groups=[[0, 1, 2, 3], [4, 5, 6, 7]], ins=[s_.ap()], outs=[d_.ap()]),
                [s_.ap()], [d_.ap()], dom="cc%d" % i_, step=1)
        for rk in range(4):
            R.dma("sp", CKA[:, :, 256 + 2048 * rk:256 + 2048 * (rk + 1)],
                  kvdst.ap()[256 * rk:256 * rk + 256, :].rearrange("(r p) c -> p r c", p=128))
            R.dma("sp", KRA[0:32, 256 + 2048 * rk:256 + 2048 * (rk + 1)], krdst.ap()[32 * rk:32 * rk + 32, :])
    tap("CKA", CKA, [2, 8448]); tap("KRA", KRA, [8448])
    if stop_after == "Hkv":
        R.emit()
        return nc, tap_out
    WKV = A.alloc([2, 2048], BF16, "WKV")
    R.dma("pool", WKV, mla_w_ukv.ap().rearrange("(r p) c -> p r c", p=128))
    for r in range(2):
        R.ts("dve", WKV[:, r, :], WKV[:, r, :], kvg[:, r:r + 1], None, ALU.mult)
    KS = A.alloc([8448], BF16, "KS")
    kblocks = [(512 * b, 512) for b in range(16)] + [(8192, 256)]
    ev = 0
    for h in range(16):
        for (c0, cn) in kblocks:
            pb = PS[ev % 4]
            ev += 1
            for r in range(2):
                R.mm(pb[0:64, 0:cn], WKV[:, r, h * 128:h * 128 + 64], CKA[:, r, c0:c0 + cn], start=(r == 0), stop=(r == 1))
            if ev % 2 == 0:
                R.cp("dve", KS[0:64, c0:c0 + cn], pb[0:64, 0:cn])
            else:
                R.cp("act", KS[0:64, c0:c0 + cn], pb[0:64, 0:cn])
        R.dma("sp", KD.ap()[h], KS[0:64, :])
    A.rel(KS)
    if stop_after == "HK":
        R.emit()
        return nc, tap_out
    VS = [A.alloc([4, 66 * 64], BF16)]
    WKVv = WKV.rearrange("p r (h t d) -> p r h t d", t=2, d=64)
    for hg in range(4):
        vs = VS[0].rearrange("p h (c d) -> p h c d", d=64)
        for c in range(66):
            pb = PS[4 + (ev % 4)]
            ev += 1
            for r in range(2):
                R.mm(pb[:, 0:256].rearrange("p (h d) -> p h d", d=64), CKA[:, r, c * 128:(c + 1) * 128],
                     WKVv[:, r, 4 * hg:4 * hg + 4, 1, :], start=(r == 0), stop=(r == 1))
            if ev % 2 == 0:
                R.cp("dve", vs[:, :, c, :], pb[:, 0:256].rearrange("p (h d) -> p h d", d=64))
            else:
                R.cp("act", vs[:, :, c, :], pb[:, 0:256].rearrange("p (h d) -> p h d", d=64))
        for hh in range(4):
            R.dma("sp", VD.ap()[4 * hg + hh], VS[0][:, hh, :])
    for x in VS + [WKV, CKA, kvg]:
        A.rel(x)
    if stop_after == "H":
        R.emit()
        return nc, tap_out

    KH = [A.alloc([8448], BF16, "KH") for _ in range(2)]
    for kb in KH:
        R.dma("sp", kb[64:96, :], KRA[0:32, :])
    A.rel(KRA)
    VH0 = A.alloc([66, 65], BF16, "VH0")
    VH1 = A.alloc([66, 128], BF16, "VH1")
    QH = [A.alloc([2048], BF16) for _ in range(2)]
    PT = [A.alloc([1024], BF16) for _ in range(3)]
    OTP = [A.alloc([1, 2048], BF16) for _ in range(2)]
    WO1 = A.alloc([8, 1024], BF16, "WO1")
    rd_sb = A.alloc([512], F32)
    bc_sb = A.alloc([512], F32)
    rd_hi = A.alloc([512], BF16)
    rd_lo = A.alloc([512], BF16)
    VST = A.alloc([66, 64], BF16, "VST")
    R.dma("pool", WO1, mla_w_out.ap().rearrange("(k p) c -> p k c", p=128))
    gate_bc(gb[0], 1, 2, 0)
    R.memset("pool", VH0, 1.0)
    R.memset("pool", VH1, 0.0)
    R.memset("pool", VH1[:, :, 0:1], 1.0)
    it = 0
    for h in range(16):
        kh, qh = KH[h % 2], QH[h % 2]
        odd = h % 2
        R.dma("sp", kh[0:64, :], KD.ap()[h])
        R.dma("sp", qh[0:96, :], QD.ap()[h])
        if odd:
            vh = VH1
            R.dma("sp", VST, VD.ap()[h])
            R.cp("pool", VH1[:, :, 64:128], VST)
            M, prow, r0, r1 = 128, 0, 64, 128
        else:
            vh = VH0
            R.dma("sp", VST, VD.ap()[h])
            R.cp("pool", VH0[:, :, 0:64], VST)
            M, prow, r0, r1 = 65, 64, 0, 64
        otp = OTP[(h // 2) % 2]
        for b in range(4):
            po = PS[6]
            it += 1
            q = qh[0:96, b * 512:(b + 1) * 512]
            for cp_ in range(33 + 2):
                if cp_ < 33:
                    T = PSP[cp_ % 3]
                    for j_ in range(2):
                        c = 2 * cp_ + j_
                        R.mm(T[:, j_ * 512:(j_ + 1) * 512], kh[0:96, c * 128:(c + 1) * 128], q)
                    R.act(PT[cp_ % 3], T[:, :], AF.Exp)
                if cp_ >= 2:
                    pp = cp_ - 2
                    for j_ in range(2):
                        cc = 2 * pp + j_
                        R.mm(po[0:M, :], vh[:, cc, 0:M], PT[pp % 3][:, j_ * 512:(j_ + 1) * 512],
                             start=(cc == 0), stop=(cc == 65))
            R.recip(rd_sb[prow:prow + 1, :], po[prow:prow + 1, :])
            R.cp("dve", rd_hi[prow:prow + 1, :], rd_sb[prow:prow + 1, :])
            R.tt("dve", rd_sb[prow:prow + 1, :], rd_sb[prow:prow + 1, :], rd_hi[prow:prow + 1, :], ALU.subtract)
            R.cp("dve", rd_lo[prow:prow + 1, :], rd_sb[prow:prow + 1, :])
            nb_ = 128 if odd else 64
            R.mm(PS[7][0:nb_, :], ones16[prow:prow + 1, 0:nb_], rd_hi[prow:prow + 1, :], start=True, stop=False)
            R.mm(PS[7][0:nb_, :], ones16[prow:prow + 1, 0:nb_], rd_lo[prow:prow + 1, :], start=False, stop=True)
            R.cp("act", bc_sb[r0:r1, :], PS[7][r0:r1, :])
            R.tt("dve", otp[r0:r1, 0, b * 512:(b + 1) * 512], po[r0:r1, :], bc_sb[r0:r1, :], ALU.mult)
        if odd:
            j = h // 2
            for t in range(2, 18):
                for nh in range(2):
                    pb = PS[6 + nh]
                    R.mm(pb[:, :], otp[:, 0, (t - 2) * 128:(t - 1) * 128], WO1[:, j, nh * 512:(nh + 1) * 512])
                    tm = tmp[t % 2][:, nh * 512:(nh + 1) * 512]
                    xs = Xt(t)[:, nh * 512:(nh + 1) * 512]
                    R.tt("dve", tm, pb[:, :], gb[0][:, nh * 512:(nh + 1) * 512], ALU.mult)
                    R.tt("pool", xs, xs, tm, ALU.add)
    for x in KH + QH + PT + OTP + [VH0, VH1, WO1, rd_sb, bc_sb, VST, rd_hi, rd_lo]:
        A.rel(x)
    tap("X3", XO, [16, 1024])
    if stop_after == "I":
        R.emit()
        return nc, tap_out

    HT2 = A.alloc([8, 2048], BF16, "HT2")
    WR = A.alloc([8, 8], F32)
    R.dma("sp", WR, moe_w_router.ap().rearrange("(k p) e -> p k e", p=128))
    brbc = A.alloc([8], F32)
    R.dma("sp", brbc, moe_b_router.ap().partition_broadcast(128))
    COMB = A.alloc([16, 8], F32, "COMB")
    xn32 = A.alloc([1024], F32)
    hT32 = A.alloc([8, 128], F32)
    junk = A.alloc([1024], BF16)
    st = A.alloc([16], F32)
    lg = A.alloc([8], F32)
    lg2 = A.alloc([8], F32)
    eq1 = A.alloc([8], F32)
    eq2 = A.alloc([8], F32)
    Af, Bf = AB[1][2], AB[1][3]
    for t in range(16):
        x = XO[:, t, :]
        ss, rs, m1, m2, dd, ee, w1, w2 = [st[:, i:i + 1] for i in range(8)]
        R.act(junk, x, AF.Square, accum_out=ss)
        R.act(rs, ss, AF.Sqrt, scale=1.0 / 1024, bias=EPS)
        R.recip(rs, rs)
        R.ts("dve", xn32, x, rs, None, ALU.mult)
        for k in range(8):
            pb = PS[k // 4]
            R.tr(pb[:, (k % 4) * 128:(k % 4 + 1) * 128], xn32[:, k * 128:(k + 1) * 128], ident32)
        for k in range(8):
            pb = PS[k // 4]
            src = pb[:, (k % 4) * 128:(k % 4 + 1) * 128]
            if k % 2 == 0:
                R.ts("dve", hT32[:, k, :], src, Af[:, k, 0:1], Bf[:, k, 0:1], ALU.mult, ALU.add)
            else:
                R.act(hT32[:, k, :], src, AF.Identity, scale=Af[:, k, 0:1], bias=Bf[:, k, 0:1])
        R.cp("pool", HT2[:, :, t * 128:(t + 1) * 128], hT32)
        pl = PS[2]
        for k in range(8):
            R.mm(pl[:, 0:8], hT32[:, k, :], WR[:, k, :], start=(k == 0), stop=(k == 7))
        R.tt("dve", lg, pl[:, 0:8], brbc, ALU.add)
        R.red(m1, lg, ALU.max)
        R.ts("dve", eq1, lg, m1, None, ALU.is_equal)
        R.stt("dve", lg2, eq1, -1e30, lg, ALU.mult, ALU.add)
        R.red(m2, lg2, ALU.max)
        R.ts("dve", eq2, lg2, m2, None, ALU.is_equal)
        R.tt("dve", dd, m2, m1, ALU.subtract)
        R.act(ee, dd, AF.Exp)
        R.ts("dve", w1, ee, 1.0, None, ALU.add)
        R.recip(w1, w1)
        R.tt("dve", w2, ee, w1, ALU.mult)
        R.ts("dve", eq1, eq1, w1, None, ALU.mult)
        R.stt("dve", COMB[:, t, :], eq2, w2, eq1, ALU.mult, ALU.add)
    for x in (xn32, hT32, junk, lg, lg2, eq1, eq2, WR, brbc):
        A.rel(x)
    tap("COMB", COMB, [16, 8])
    gate_bc(gb[0], 1, 5, 0)
    ne = 8 if stop_after != "J1" else 1
    for e in range(ne):
        ffn(HT2, 2048, [(t, (t - 2) * 128, 0) for t in range(2, 18)],
            moe_w_gate.ap()[e], moe_w_up.ap()[e], moe_w_down.ap()[e], 28, gb,
            comb=lambda t, e=e: COMB[:, t - 2, e:e + 1])
    A.rel(HT2)

    fgbc = A.alloc([1024], F32)
    R.dma("sp", fgbc, final_g.ap().partition_broadcast(128))
    ot = [A.alloc([1024], F32) for _ in range(2)]
    junk = A.alloc([1024], BF16)
    for t in range(16):
        x = XO[:, t, :]
        ss, rs = st[:, 8:9], st[:, 9:10]
        R.act(junk, x, AF.Square, accum_out=ss)
        R.act(rs, ss, AF.Sqrt, scale=1.0 / 1024, bias=EPS)
        R.recip(rs, rs)
        R.stt("dve", ot[t % 2], x, rs, fgbc, ALU.mult, ALU.mult)
        R.dma("sp", out_d.ap()[t * 128:(t + 1) * 128, :], ot[t % 2])
    R.emit()
    return nc, tap_out


def _na_bias_tables(rpb, cpos):
    NEG = np.float32(-30000.0)
    tab = np.full((8, 27, 128, 128), NEG, np.float32)
    rows_b = 128

    def cfg(p):
        if p == 0:
            return list(range(0, 6)), 0
        if p == 1:
            return list(range(1, 6)), 6
        if p == 14:
            return list(range(14, 19)), 16
        if p == 15:
            return list(range(14, 20)), 21
        return list(range(p, p + 5)), 11

    cols = np.arange(64)
    cstart = np.clip(cols - 8, 0, 48)
    for p in (0, 1, 2, 14, 15):
        chunks, slot0 = cfg(p)
        for s, m in enumerate(chunks):
            for kr_l in range(2):
                jrow = 2 * m + kr_l
                grow = 32 * cpos - 4 + jrow
                for qr_l in range(2):
                    i = 2 * p + qr_l
                    r = 32 * cpos + i
                    rs = min(max(r - 4, 0), rows_b - 8)
                    if grow < rs or grow >= rs + 8 or grow < 0 or grow >= rows_b:
                        continue
                    ridx = grow - r + 7
                    kc = cols[:, None]
                    qc = cols[None, :]
                    valid = (kc >= cstart[None, :]) & (kc < cstart[None, :] + 16)
                    cidx = np.clip(kc - qc + 15, 0, 30)
                    vals = rpb[:, ridx, :][:, cidx]
                    blk = np.where(valid[None], vals, NEG)
                    tab[:, slot0 + s, kr_l * 64:(kr_l + 1) * 64, qr_l * 64:(qr_l + 1) * 64] = blk
    return np.ascontiguousarray(tab.transpose(0, 2, 1, 3))


def _rope_tables(cpos):
    t = np.arange(2048, dtype=np.int64) + 2048 * cpos
    pos = np.stack([t // GRID_W, t % GRID_W], -1).astype(np.float32)
    inv = np.power(np.float32(10000.0), -np.arange(8, dtype=np.float32) / np.float32(8)).astype(np.float32)
    ang = pos[:, :, None] * inv
    cos = np.cos(ang).astype(np.float32)
    sin = np.sin(ang).astype(np.float32)
    C = np.zeros((32, 2048), np.float32)
    S = np.zeros((32, 2048), np.float32)
    for a in range(2):
        for half in range(2):
            rows = slice(a * 16 + half * 8, a * 16 + half * 8 + 8)
            C[rows] = cos[:, a, :].T
            S[rows] = (-sin[:, a, :].T) if half == 0 else sin[:, a, :].T
    scale = np.float32(96.0 ** -0.5)
    Cq = np.concatenate([np.full((64, 2048), scale, np.float32), C * scale], 0)
    Sq = np.concatenate([np.zeros((64, 2048), np.float32), S * scale], 0)
    return np.stack([Cq, Sq]), np.stack([C, S])


_SWAP32 = np.concatenate([np.arange(8, 16), np.arange(0, 8), np.arange(24, 32), np.arange(16, 24)])


def _rope_k_full():
    t = np.arange(8192, dtype=np.int64)
    pos = np.stack([t // GRID_W, t % GRID_W], -1).astype(np.float32)
    inv = np.power(np.float32(10000.0), -np.arange(8, dtype=np.float32) / np.float32(8)).astype(np.float32)
    ang = pos[:, :, None] * inv
    cos = np.cos(ang).astype(np.float32)
    sin = np.sin(ang).astype(np.float32)
    C = np.zeros((32, 8192), np.float32)
    S = np.zeros((32, 8192), np.float32)
    for a in range(2):
        for half in range(2):
            rows = slice(a * 16 + half * 8, a * 16 + half * 8 + 8)
            C[rows] = cos[:, a, :].T
            S[rows] = (-sin[:, a, :].T) if half == 0 else sin[:, a, :].T
    return np.stack([C, S])


_f = lambda a: np.ascontiguousarray(np.asarray(a, dtype=np.float32))


def make_maps_l0(inp):
    f = _f
    x = f(inp["x"]); ctxa = f(inp["ctx"])
    shared = dict(
        ada_w=f(inp["ada_w"]), ada_b=f(inp["ada_b"]), norm_g=f(inp["norm_g"]),
        na_w_in=f(inp["na_w_in"][0]), sg_w=f(inp["sg_w"][0]), sg_b=f(inp["sg_b"][0]), sg_norm_g=f(inp["sg_norm_g"][0]),
        even_w_out=f(inp["even_w_out"][0]), ffn_w_gate=f(inp["ffn_w_gate"][0]), ffn_w_up=f(inp["ffn_w_up"][0]),
        ffn_w_down=f(inp["ffn_w_down"][0]), ident=np.eye(128, dtype=np.float32),
    )
    rpb = f(inp["na_rpb"][0])
    maps = []
    for c in range(8):
        b, cp = c // 4, c % 4
        xhp = np.zeros((2560, 1024), np.float32)
        lo = 2048 * cp - 256
        s0, s1 = max(lo, 0), min(lo + 2560, 8192)
        xhp[s0 - lo:s1 - lo] = x[b, s0:s1]
        m = dict(shared)
        m.update(xh=xhp, ctx=ctxa[b], cond=np.stack([f(inp["c"])[b], f(inp["c_ctx"])]),
                 nabias=_na_bias_tables(rpb, cp))
        maps.append(m)
    return maps


def make_maps_l1(inp, x2, c2, fused=False):
    f = _f
    uq = f(inp["mla_w_uq"][0])
    perm = np.arange(1536).reshape(16, 96).copy()
    perm[:, 64:] = perm[:, 64:][:, _SWAP32]
    uq_sw = np.ascontiguousarray(uq[:, perm.reshape(-1)])
    w_in1 = f(inp["mla_w_in"][0])
    kr_sw = np.ascontiguousarray(w_in1[:, 640:672][:, _SWAP32])
    shared = dict(
        ada_w=f(inp["ada_w"]), ada_b=f(inp["ada_b"]), norm_g=f(inp["norm_g"]), final_g=f(inp["final_g"]),
        mla_w_in=w_in1, mla_w_kr_sw=kr_sw,
        mla_q_norm_g=f(inp["mla_q_norm_g"][0]), mla_kv_norm_g=f(inp["mla_kv_norm_g"][0]),
        mla_w_uq=uq, mla_w_uq_sw=uq_sw, mla_w_ukv=f(inp["mla_w_ukv"][0]), mla_w_out=f(inp["mla_w_out"][0]),
        moe_w_router=f(inp["moe_w_router"][0]), moe_b_router=f(inp["moe_b_router"][0]),
        moe_w_gate=f(inp["moe_w_gate"][0]), moe_w_up=f(inp["moe_w_up"][0]), moe_w_down=f(inp["moe_w_down"][0]),
        ident=np.eye(128, dtype=np.float32), ropek=_rope_k_full(),
    )
    maps = []
    for c in range(8):
        b, cp = c // 4, c % 4
        rq, rk_ = _rope_tables(cp)
        m = dict(shared)
        if fused:
            m.update(ropek=rk_, ropeq=rq)
        else:
            m.update(xb=np.ascontiguousarray(x2[b]), xo=np.ascontiguousarray(x2[b, 2048 * cp:2048 * (cp + 1)]),
                     ctx=np.ascontiguousarray(c2[b]), cond=np.stack([f(inp["c"])[b], f(inp["c_ctx"])]), ropeq=rq)
        maps.append(m)
    return maps


def make_maps_fused(inp):
    m0 = make_maps_l0(inp)
    x = _f(inp["x"]); ctxa = _f(inp["ctx"])
    m1 = make_maps_l1(inp, x, ctxa, fused=True)
    maps = []
    for c in range(8):
        m = dict(m1[c])
        m.update(m0[c])
        maps.append(m)
    return maps


def kernel(**inputs):
    nc, _ = build("fused")
    res = run_bass_kernel_spmd(nc, make_maps_fused(inputs), core_ids=list(range(8)))
    out = np.zeros((2, 8192, 1024), np.float32)
    for c in range(8):
        b, cp = c // 4, c % 4
        out[b, 2048 * cp:2048 * (cp + 1)] = res.results[c]["out"]
    return out
```
